# Optimizing a Trainium2 kernel written in Bass

```python
import jax, jax.numpy as jnp
from jax import lax
import numpy as np

D_MODEL = 1024
BATCH = 8
SEQ = 2048
DEPTH = 2

HEAD_DIM = 64
ATTN_WIDTH = D_MODEL // 2
ATTN_HEADS = ATTN_WIDTH // HEAD_DIM
CONV_WIDTH = D_MODEL // 4
CONV_HEADS = CONV_WIDTH // HEAD_DIM
CONV_K = 3
POOL_WIDTH = D_MODEL // 4
POOL_WINDOWS = (2, 4, 8, 16)
POOL_GROUPS = len(POOL_WINDOWS)
POOL_GROUP_DIM = POOL_WIDTH // POOL_GROUPS
MIX_WIDTH = ATTN_WIDTH + CONV_WIDTH + POOL_WIDTH
IN_WIDTH = 3 * ATTN_WIDTH + 3 * CONV_WIDTH + POOL_WIDTH
MOBA_BLOCK = 256
MOBA_TOPK = 3
Q_CHUNK = 32
D_FF = -(-(8 * D_MODEL) // (3 * 256)) * 256
NORM_EPS = 1e-6

kernel_name = "hybrid_moba_conv_pool_block"


def rms_norm(x, g):
    xf = x.astype(jnp.float32)
    y = xf * lax.rsqrt(jnp.mean(xf * xf, axis=-1, keepdims=True) + NORM_EPS)
    return (y * g.astype(jnp.float32)).astype(x.dtype)


def alibi_slopes(n_heads):
    return jnp.asarray(2.0 ** (-8.0 * np.arange(1, n_heads + 1) / n_heads), dtype=jnp.float32)


def moba_attention(q, k, v):
    B, H, S, Dh = q.shape
    nb = -(-S // MOBA_BLOCK)
    s_pad = nb * MOBA_BLOCK
    topk = min(MOBA_TOPK, nb)
    pad = ((0, 0), (0, 0), (0, s_pad - S), (0, 0))
    kp = jnp.pad(k, pad)
    vp = jnp.pad(v, pad)
    k_blocks = kp.reshape(B, H, nb, MOBA_BLOCK, Dh)
    v_blocks = vp.reshape(B, H, nb, MOBA_BLOCK, Dh)
    k_mean = jnp.mean(k_blocks.astype(jnp.float32), axis=3)
    slopes = alibi_slopes(H)
    scale = Dh ** -0.5
    b_idx = jnp.arange(B)[:, None, None]
    h_idx = jnp.arange(H)[None, :, None]
    blk_pos = jnp.arange(MOBA_BLOCK)
    neg_inf = jnp.float32(-jnp.inf)

    def chunk(start):
        qc = lax.dynamic_slice_in_dim(q, start, Q_CHUNK, axis=2).astype(jnp.float32)
        t = start + jnp.arange(Q_CHUNK)
        cur = start // MOBA_BLOCK
        gate = jnp.einsum('bhqd,bhnd->bhqn', qc, k_mean)
        gate = jnp.where(jnp.arange(nb) < cur, gate, neg_inf)
        _, gidx = lax.top_k(gate, topk)
        valid = jnp.arange(topk) < cur
        own_start = cur * MOBA_BLOCK
        k_own = lax.dynamic_slice_in_dim(kp, own_start, MOBA_BLOCK, axis=2)
        v_own = lax.dynamic_slice_in_dim(vp, own_start, MOBA_BLOCK, axis=2)
        s_own = own_start + blk_pos
        dist_own = (t[:, None] - s_own[None, :]).astype(jnp.float32)
        logit_own = (jnp.einsum('bhqd,bhkd->bhqk', qc, k_own) * scale
                     - slopes[:, None, None] * dist_own)
        logit_own = jnp.where(dist_own >= 0, logit_own, neg_inf)
        flat_idx = gidx.reshape(B, H, Q_CHUNK * topk)
        k_sel = k_blocks[b_idx, h_idx, flat_idx].reshape(B, H, Q_CHUNK, topk, MOBA_BLOCK, Dh)
        v_sel = v_blocks[b_idx, h_idx, flat_idx].reshape(B, H, Q_CHUNK, topk, MOBA_BLOCK, Dh)
        s_sel = gidx[..., None] * MOBA_BLOCK + blk_pos
        dist_sel = (t[None, None, :, None, None] - s_sel).astype(jnp.float32)
        logit_sel = (jnp.einsum('bhqd,bhqnkd->bhqnk', qc, k_sel) * scale
                     - slopes[None, :, None, None, None] * dist_sel)
        logit_sel = jnp.where(valid[:, None], logit_sel, neg_inf)
        logits = jnp.concatenate(
            [logit_own, logit_sel.reshape(B, H, Q_CHUNK, topk * MOBA_BLOCK)], axis=-1)
        p = jax.nn.softmax(logits, axis=-1)
        p_own = p[..., :MOBA_BLOCK]
        p_sel = p[..., MOBA_BLOCK:].reshape(B, H, Q_CHUNK, topk, MOBA_BLOCK)
        o = (jnp.einsum('bhqk,bhkd->bhqd', p_own, v_own)
             + jnp.einsum('bhqnk,bhqnkd->bhqd', p_sel, v_sel))
        return o.astype(q.dtype)

    starts = jnp.arange(S // Q_CHUNK) * Q_CHUNK
    out = lax.map(chunk, starts)
    return out.transpose(1, 0, 3, 2, 4).reshape(B, S, H * Dh)


def short_conv_mixer(h, b_gate, c_gate, conv_w):
    S = h.shape[1]
    u = c_gate * h
    up = jnp.pad(u, ((0, 0), (CONV_K - 1, 0), (0, 0)))
    conv = up[:, 0:S] * conv_w[0]
    for j in range(1, CONV_K):
        conv = conv + up[:, j:j + S] * conv_w[j]
    return b_gate * conv


def pool_mixer(u, pool_w, pool_scale):
    B, S, _ = u.shape
    uf = u.astype(jnp.float32).reshape(B, S, POOL_GROUPS, POOL_GROUP_DIM)
    cs = jnp.cumsum(uf, axis=1)
    t = jnp.arange(1, S + 1, dtype=jnp.float32)
    outs = []
    for g, w in enumerate(POOL_WINDOWS):
        csg = cs[:, :, g]
        lag = jnp.pad(csg, ((0, 0), (w, 0), (0, 0)))[:, :S]
        cnt = jnp.minimum(t, jnp.float32(w))[None, :, None]
        outs.append((csg - lag) / cnt - uf[:, :, g])
    pooled = jnp.stack(outs, axis=2).astype(u.dtype)
    y = jnp.einsum('bsgc,gcd->bsgd', pooled, pool_w)
    return y.reshape(B, S, POOL_WIDTH) * pool_scale


def hybrid_layer(x, w_in, w_out, conv_w, pool_w, pool_scale,
                 g_pre_mix, g_post_mix, g_pre_ffn, g_post_ffn, w_gate, w_up, w_down):
    B, S, _ = x.shape
    h = rms_norm(x, g_pre_mix)
    proj = h @ w_in
    offs = np.cumsum([ATTN_WIDTH, ATTN_WIDTH, ATTN_WIDTH, CONV_WIDTH, CONV_WIDTH, CONV_WIDTH]).tolist()
    q, k, v, h_conv, b_gate, c_gate, u_pool = jnp.split(proj, offs, axis=-1)
    to_heads = lambda a: a.reshape(B, S, ATTN_HEADS, HEAD_DIM).transpose(0, 2, 1, 3)
    attn = moba_attention(to_heads(q), to_heads(k), to_heads(v))
    conv = short_conv_mixer(h_conv, b_gate, c_gate, conv_w)
    pool = pool_mixer(u_pool, pool_w, pool_scale)
    mixed = jnp.concatenate([attn, conv, pool], axis=-1) @ w_out
    x = x + rms_norm(mixed, g_post_mix)
    hf = rms_norm(x, g_pre_ffn)
    ff = (jax.nn.silu(hf @ w_gate) * (hf @ w_up)) @ w_down
    return x + rms_norm(ff, g_post_ffn)


def setup_inputs(seed: int = 0) -> dict:
    key = jax.random.key(seed)
    ks = jax.random.split(key, 14)
    f32 = jnp.float32
    nrm = lambda k, shape, s: jax.random.normal(k, shape, f32) * s
    gain = lambda k: 1.0 + 0.05 * jax.random.normal(k, (DEPTH, D_MODEL), f32)
    return {
        "x": jax.random.normal(ks[0], (BATCH, SEQ, D_MODEL), f32),
        "w_in": nrm(ks[1], (DEPTH, D_MODEL, IN_WIDTH), D_MODEL ** -0.5),
        "w_out": nrm(ks[2], (DEPTH, MIX_WIDTH, D_MODEL), MIX_WIDTH ** -0.5),
        "conv_w": nrm(ks[3], (DEPTH, CONV_K, CONV_WIDTH), CONV_K ** -0.5),
        "pool_w": nrm(ks[4], (DEPTH, POOL_GROUPS, POOL_GROUP_DIM, POOL_GROUP_DIM), POOL_GROUP_DIM ** -0.5),
        "pool_scale": 1.0 + 0.1 * jax.random.normal(ks[5], (DEPTH, POOL_WIDTH), f32),
        "g_pre_mix": gain(ks[6]),
        "g_post_mix": gain(ks[7]),
        "g_pre_ffn": gain(ks[8]),
        "g_post_ffn": gain(ks[9]),
        "w_gate": nrm(ks[10], (DEPTH, D_MODEL, D_FF), D_MODEL ** -0.5),
        "w_up": nrm(ks[11], (DEPTH, D_MODEL, D_FF), D_MODEL ** -0.5),
        "w_down": nrm(ks[12], (DEPTH, D_FF, D_MODEL), D_FF ** -0.5),
    }


def reference(x, w_in, w_out, conv_w, pool_w, pool_scale,
              g_pre_mix, g_post_mix, g_pre_ffn, g_post_ffn, w_gate, w_up, w_down):
    for l in range(DEPTH):
        x = hybrid_layer(x, w_in[l], w_out[l], conv_w[l], pool_w[l], pool_scale[l],
                         g_pre_mix[l], g_post_mix[l], g_pre_ffn[l], g_post_ffn[l],
                         w_gate[l], w_up[l], w_down[l])
    return x
```

```python
import contextlib
import numpy as np
import ml_dtypes
import concourse.bass as bass
import concourse.mybir as mybir
from concourse.bass_utils import run_bass_kernel_spmd

F32 = mybir.dt.float32
BF16 = mybir.dt.bfloat16
U8 = mybir.dt.uint8
FP8 = mybir.dt.float8e4
AF = mybir.ActivationFunctionType
ALU = mybir.AluOpType
AX = mybir.AxisListType

D = 1024
S = 2048
NT = 16
DEPTH = 2
DFF = 2816
NF = 22
EPS = 1e-6
NEG = -30000.0
ENGS = ("pe", "act", "dve", "pool", "sp")
STRICT_SAME_ENGINE = True


_ES = {"float8e4": 1, "float32": 4, "bfloat16": 2, "uint8": 1, "int32": 4, "uint32": 4, "float16": 2, "uint16": 2}


class Tracker:
    def __init__(self, nc):
        self.nc = nc
        self.ops = []
        self.idx = {}
        self.known = {e: {} for e in ENGS}
        self.clock = {}
        self.sb = {}
        self.ps = {}
        self.G = 512
        self.dma_keys = set()

    @staticmethod
    def _box(ap):
        t = ap.tensor
        tn = type(t).__name__
        if tn.startswith("SB"):
            space = "sb"
        elif tn.startswith("PS"):
            space = "ps"
        else:
            return None
        es = _ES[str(ap.dtype).split(".")[-1]]
        dims = list(ap.ap)
        pstride, pcount = dims[0]
        off = ap.offset
        p0 = off // pstride if pstride else 0
        f0 = off - p0 * pstride
        lo = hi = f0
        for st, cnt in dims[1:]:
            if st >= 0:
                hi += st * (cnt - 1)
            else:
                lo += st * (cnt - 1)
        return (space, p0, p0 + pcount, lo * es, (hi + 1) * es)

    def _conflicts(self, box, mode):
        space, p0, p1, b0, b1 = box
        out = []
        if space == "ps":
            for bank in range(b0 // 2048, (b1 - 1) // 2048 + 1):
                out.extend(self.ps.get(bank, ()))
            return out
        seen = set()
        for g in range(b0 // self.G, (b1 - 1) // self.G + 1):
            for r in self.sb.get(g, ()):
                if id(r) in seen:
                    continue
                rp0, rp1, rb0, rb1, rmode = r[0], r[1], r[2], r[3], r[4]
                if rb0 < b1 and b0 < rb1 and rp0 < p1 and p0 < rp1 and (mode == "w" or rmode == "w"):
                    seen.add(id(r))
                    out.append(r)
        return out

    def _register(self, box, mode, key, idx, eng):
        space, p0, p1, b0, b1 = box
        if space == "ps":
            rec = (0, 128, 0, 0, mode, key, idx, eng)
            for bank in range(b0 // 2048, (b1 - 1) // 2048 + 1):
                self.ps[bank] = [rec]
            return
        rec = (p0, p1, b0, b1, mode, key, idx, eng)
        G = self.G
        for g in range(b0 // G, (b1 - 1) // G + 1):
            lst = self.sb.get(g)
            if lst is None:
                self.sb[g] = [rec]
                continue
            new = []
            for r in lst:
                rp0, rp1, rb0, rb1, rmode, rkey = r[0], r[1], r[2], r[3], r[4], r[5]
                gb0 = max(rb0, g * G)
                gb1 = min(rb1, (g + 1) * G)
                covered = (p0 <= rp0 and rp1 <= p1 and b0 <= gb0 and gb1 <= b1)
                if covered and (mode == "w" or (rmode == "r" and rkey == key)):
                    continue
                new.append(r)
            new.append(rec)
            self.sb[g] = new

    def op(self, eng, fn, reads=(), writes=(), dma=None):
        key = dma if dma is not None else eng
        if dma is not None:
            self.dma_keys.add(dma)
        idx = self.idx.get(key, 0) + 1
        self.idx[key] = idx
        known = self.known[eng]
        need = {}
        rboxes = [b for b in (self._box(a) for a in reads) if b is not None]
        wboxes = [b for b in (self._box(a) for a in writes) if b is not None]
        for boxes, mode in ((rboxes, "r"), (wboxes, "w")):
            for box in boxes:
                for r in self._conflicts(box, mode):
                    rmode, rkey, ridx = r[4], r[5], r[6]
                    if rkey == key:
                        if dma is not None:
                            pass
                        elif eng == "pe":
                            continue
                        elif STRICT_SAME_ENGINE or (rmode == "w" and mode == "r"):
                            pass
                        else:
                            continue
                    if rkey in self.dma_keys:
                        ridx = self.idx[rkey] if rkey != key else idx - 1
                    if need.get(rkey, 0) < ridx:
                        need[rkey] = ridx
        waits = []
        for k, v in need.items():
            if v <= 0 or known.get(k, 0) >= v:
                continue
            waits.append((k, v))
            known[k] = v
            ck = self.clock.get((k, v))
            if ck:
                for kk, vv in ck.items():
                    if known.get(kk, 0) < vv:
                        known[kk] = vv
        snap = dict(known)
        snap[key] = idx
        self.clock[(key, idx)] = snap
        for box in rboxes:
            self._register(box, "r", key, idx, eng)
        for box in wboxes:
            self._register(box, "w", key, idx, eng)
        self.ops.append([eng, key, idx, fn, waits, dma is not None])
        return (key, idx)

    def wait_all(self, eng, tokens):
        self.ops.append([eng, None, None, None, list(tokens), False])

    def emit(self):
        nc = self.nc
        sig = {}
        for eng, key, idx, fn, waits, is_dma in self.ops:
            for k, v in waits:
                sig.setdefault(k, set()).add(v)
        dma_keys = self.dma_keys
        cnt_of = {}
        for k, s in sig.items():
            if k in dma_keys:
                continue
            for n, v in enumerate(sorted(s)):
                cnt_of[(k, v)] = n + 1
        keys = sorted(set(sig.keys()) | set(dma_keys))
        sems = {}
        with contextlib.ExitStack() as st:
            for k in keys:
                sems[k] = st.enter_context(nc.semaphore("s_" + k))
            block = st.enter_context(nc.Block())
            per_eng = {e: [] for e in ENGS}
            for o in self.ops:
                per_eng[o[0]].append(o)

            def body(e, eng):
                for _, key, idx, fn, waits, is_dma in per_eng[eng]:
                    for k, v in waits:
                        val = 16 * v if k in dma_keys else cnt_of[(k, v)]
                        e.wait_ge(sems[k], val)
                    if fn is None:
                        continue
                    ins = fn(e)
                    if is_dma:
                        ins.then_inc(sems[key], 16)
                    elif (key, idx) in cnt_of:
                        ins.then_inc(sems[key], 1)

            @block.tensor
            def _(e):
                body(e, "pe")

            @block.scalar
            def _(e):
                body(e, "act")

            @block.vector
            def _(e):
                body(e, "dve")

            @block.gpsimd
            def _(e):
                body(e, "pool")

            @block.sync
            def _(e):
                body(e, "sp")
        self.stats = {e: len(v) for e, v in per_eng.items()}
        self.stats["waits"] = sum(len(o[4]) for o in self.ops)
        self.stats["sems"] = len(keys)


X_OFF = 0
H_OFF = 65536
R2_OFF = 98304
RING_OFF = 188416
C_OFF = 204800
MEM_BYTES = 212800

CONCAT_OFF = R2_OFF + 0
V_OFF = R2_OFF + 32768
B1_OFF = R2_OFF + 32768
QK_OFF = R2_OFF + 57344
PT_OFF = R2_OFF + 73728
RDEN_OFF = R2_OFF + 77824
MISC_OFF = R2_OFF + 81920
ACTT_OFF = R2_OFF + 0
WD_OFF = R2_OFF + 45056
HU_OFF = H_OFF + 16384
GBC_OFF = C_OFF + 0
IDENT_OFF = C_OFF + 4096
TRI_OFF = C_OFF + 4352
SBIAS_OFF = C_OFF + 4608
GM_OFF = C_OFF + 4864
TOP8_OFF = C_OFF + 5120
SELB_OFF = C_OFF + 5376
BIASB_OFF = C_OFF + 5632
KMT_OFF = C_OFF + 5888
STAT_OFF = C_OFF + 5920
CONVW_OFF = C_OFF + 6368
PSCALE_OFF = C_OFF + 6392
RC_OFF = C_OFF + 6400
POOLW_OFF = C_OFF + 6528
POWC_OFF = C_OFF + 7040
GTA_OFF = C_OFF + 7044
GTF_OFF = C_OFF + 7076
KMF_OFF = C_OFF + 7108


def build_nc(n_layers=DEPTH, first_layer=0, dbg=()):
    nc = bass.Bass("TRN2", target_bir_lowering=False)
    L = DEPTH

    def din(name, shape, dt=F32):
        return nc.dram_tensor(name, list(shape), dt, kind="ExternalInput").ap()

    x_d = din("x", [S, D])
    wcp_d = din("wcp", [L, 4, 128, 2048])
    wv_d = din("wv", [L, 2, 128, 2048])
    wqk_d = din("wqk", [L, 4, 128, 2048])
    wout_d = din("wout", [L, 4, 128, 2048])
    wgu_d = din("wgu", [L, NF, 128, 2048])
    wd_d = din("wd", [L, 11, 128, 2048])
    gpost_d = din("gpost", [L, 2, D])
    gtpre_d = din("gtpre", [L, 2, 128, 8])
    convw_d = din("convw", [L, 128, 6])
    pscale_d = din("pscale", [L, 128, 2])
    poolw_d = din("poolw", [L, 128, 256])
    ident_d = din("ident", [128, 128], BF16)
    tri_d = din("tri", [128, 128], BF16)
    kaug_d = din("kaug", [12, S], BF16)
    qaug_d = din("qaug", [8, 12, S], BF16)
    sbias_d = din("sbias", [128, 64])
    rc_d = din("rc", [128, 32])
    y_d = nc.dram_tensor("y", [S, D], F32, kind="ExternalOutput").ap()
    dbg_out = {}
    for name, shape, dt in dbg:
        dbg_out[name] = nc.dram_tensor("dbg_" + name, list(shape), dt, kind="ExternalOutput").ap()

    with contextlib.ExitStack() as st:
        mem = st.enter_context(nc.sbuf_tensor("mem", [128, MEM_BYTES], U8))
        ps = st.enter_context(nc.psum_tensor("ps", [128, 8, 512], F32))
        T = Tracker(nc)
        memb = mem.bitcast(BF16)

        def sbv(off, shape, dt):
            es = 4 if dt == F32 else 2
            n = int(np.prod(shape))
            v = mem[:, off:off + n * es].bitcast(dt)
            if len(shape) == 2:
                v = v.rearrange("p (a b) -> p a b", b=shape[1])
            elif len(shape) == 3:
                v = v.rearrange("p (a b c) -> p a b c", b=shape[1], c=shape[2])
            return v

        X = sbv(X_OFF, (NT, D), F32)
        H = sbv(H_OFF, (8, S), BF16)
        HF = sbv(H_OFF, (8, 1024), BF16)
        CONCAT = sbv(CONCAT_OFF, (8, S), BF16)
        V = sbv(V_OFF, (NT, 4, 192), BF16)
        QT = [sbv(QK_OFF + 0, (S,), BF16), sbv(QK_OFF + 4096, (S,), BF16)]
        KT = [sbv(QK_OFF + 8192, (S,), BF16), sbv(QK_OFF + 12288, (S,), BF16)]
        TS_ = 2112
        UCV = [sbv(B1_OFF + m * TS_, (528,), F32) for m in range(2)]
        UPL = [sbv(B1_OFF + (2 + m) * TS_, (528,), F32) for m in range(2)]
        CSC = [[[sbv(B1_OFF + (4 + (par * 2 + m) * 3 + k) * TS_, (528,), F32) for k in range(3)]
                for m in range(2)] for par in range(2)]
        PSC = [[sbv(B1_OFF + (16 + m * 2 + k) * TS_, (528,), F32) for k in range(2)] for m in range(2)]
        PLB = [[sbv(B1_OFF + 20 * TS_ + (par * 2 + m) * 1024, (512,), BF16) for m in range(2)] for par in range(2)]
        TD = sbv(MISC_OFF + 6144, (16,), F32)
        PT = [sbv(PT_OFF + 1024 * j, (512,), BF16) for j in range(4)]
        RDEN = [sbv(RDEN_OFF + 2048 * j, (512,), F32) for j in range(2)]
        XS_A = [sbv(MISC_OFF + 2048 * j, (D,), BF16) for j in range(2)]
        JUNK_A = [mem[:, MISC_OFF + 4096 + 1024 * j:MISC_OFF + 4096 + 1024 * (j + 1)] for j in range(2)]
        ACTT = sbv(ACTT_OFF, (NF, 1024), BF16)
        WD = sbv(WD_OFF, (NF, 1024), BF16)
        XS_F = [sbv(HU_OFF + 2048 * j, (D,), BF16) for j in range(2)]
        JUNK_F = [mem[:, HU_OFF + 4096 + 1024 * j:HU_OFF + 4096 + 1024 * (j + 1)] for j in range(2)]
        SG = [sbv(HU_OFF + 6144 + 1024 * j, (512,), BF16) for j in range(2)]
        TMP = [sbv(HU_OFF + 8192 + 2048 * j, (512,), F32) for j in range(2)]
        WCP = sbv(RING_OFF, (8, 1024), BF16)
        WV = sbv(RING_OFF, (8, 512), BF16)
        WQK = [sbv(RING_OFF + 8192 + 4096 * j, (2, 8, 128), BF16) for j in range(2)]
        WOUT = sbv(RING_OFF, (8, 1024), BF16)
        NS = 4
        GU = [sbv(RING_OFF + 4096 * j, (2, 8, 128), BF16) for j in range(NS)]
        GBC = sbv(GBC_OFF, (D,), F32)
        IDENT = sbv(IDENT_OFF, (128,), BF16)
        TRI = sbv(TRI_OFF, (128,), BF16)
        SBIAS = sbv(SBIAS_OFF, (64,), F32)
        GM = sbv(GM_OFF, (64,), F32)
        TOP8 = sbv(TOP8_OFF, (64,), F32)
        SELB = sbv(SELB_OFF, (64,), F32)
        BIASB = sbv(BIASB_OFF, (128,), BF16)
        KMT = [sbv(KMT_OFF + 16 * j, (8,), BF16) for j in range(2)]
        KMF = [sbv(KMF_OFF + 32 * j, (8,), F32) for j in range(2)]
        STAT = sbv(STAT_OFF, (112,), F32)
        SS, MS, RSTD = STAT[:, 0:16], STAT[:, 16:32], STAT[:, 32:48]
        SS2, MS2, RSTD2 = STAT[:, 48:80], STAT[:, 80:96], STAT[:, 96:112]
        CONVW = sbv(CONVW_OFF, (2, 3), F32)
        PSCALE = sbv(PSCALE_OFF, (2,), F32)
        RC = sbv(RC_OFF, (2, 16), F32)
        POOLW = sbv(POOLW_OFF, (2, 128), BF16)
        EPSB = sbv(POWC_OFF, (1,), F32)
        GTA = sbv(GTA_OFF, (8,), F32)
        GTF = sbv(GTF_OFF, (8,), F32)

        def vaug(i, h):
            lo = 0 if h % 2 == 0 else 64
            return V[:, i, h // 2, lo:lo + 128]

        def isap(v):
            return hasattr(v, "tensor") and hasattr(v, "ap")

        def mm(out, lhsT, rhs, start, stop):
            T.op("pe", lambda e: e.matmul(out, lhsT=lhsT, rhs=rhs, start=start, stop=stop,
                                          skip_group_check=True),
                 reads=[lhsT, rhs], writes=[out])

        def tr(out, in_):
            T.op("pe", lambda e: e.transpose(out=out, in_=in_, identity=IDENT),
                 reads=[in_, IDENT], writes=[out])

        def act(out, in_, func, scale=None, accum=None, bias=None):
            kw = {}
            if bias is not None:
                kw["bias"] = bias
            rd = [in_]
            wr = [out]
            if scale is not None:
                kw["scale"] = scale
                if isap(scale):
                    rd.append(scale)
            if accum is not None:
                kw["accum_out"] = accum
                wr.append(accum)
            T.op("act", lambda e: e.activation(out=out, in_=in_, func=func, **kw), reads=rd, writes=wr)

        def tt(eng, out, in0, in1, op):
            T.op(eng, lambda e: e.tensor_tensor(out=out, in0=in0, in1=in1, op=op),
                 reads=[in0, in1], writes=[out])

        def ts(eng, out, in0, s1, s2, op0, op1=None):
            rd = [in0] + [s for s in (s1, s2) if isap(s)]
            if op1 is None:
                T.op(eng, lambda e: e.tensor_scalar(out=out, in0=in0, scalar1=s1, scalar2=None, op0=op0),
                     reads=rd, writes=[out])
            else:
                T.op(eng, lambda e: e.tensor_scalar(out=out, in0=in0, scalar1=s1, scalar2=s2, op0=op0, op1=op1),
                     reads=rd, writes=[out])

        def stt(out, in0, scalar, in1, op0, op1):
            rd = [in0, in1] + ([scalar] if isap(scalar) else [])
            T.op("dve", lambda e: e.scalar_tensor_tensor(out=out, in0=in0, scalar=scalar, in1=in1, op0=op0, op1=op1),
                 reads=rd, writes=[out])

        def copy(eng, out, in_):
            if eng == "act":
                act(out, in_, AF.Copy)
            else:
                T.op(eng, lambda e: e.tensor_copy(out=out, in_=in_), reads=[in_], writes=[out])

        def memset(eng, out, val):
            T.op(eng, lambda e: e.memset(out, val), writes=[out])

        def dma(q, out, in_, stream):
            return T.op(q, lambda e: e.dma_start(out=out, in_=in_), reads=[in_], writes=[out], dma=stream)

        bank_ctr = [0]

        def nb():
            b = bank_ctr[0] % 8
            bank_ctr[0] += 1
            return b

        alt_ctr = [0]

        def alt():
            alt_ctr[0] += 1
            return "act" if alt_ctr[0] % 2 else "dve"

        def dump(name, src):
            if name in dbg_out:
                dma("sp", dbg_out[name], src, "dbg_" + name)

        dma("sp", IDENT, ident_d[:, :], "c_ident")
        dma("sp", GTA, gtpre_d[first_layer, 0], "p_gta")
        for i in range(4):
            dma("sp", X[:, i, :], x_d[i * 128:(i + 1) * 128, :], "x%d" % i)
        dma("sp", TRI, tri_d[:, :], "c_tri")
        dma("sp", SBIAS, sbias_d[:, :], "c_sbias")
        dma("sp", RC.rearrange("p a b -> p (a b)"), rc_d[:, :], "c_rc")
        memset("dve", BIASB[:, 0:64], 0.0)
        memset("dve", EPSB, EPS)

        pn_ctr = [0]

        def prenorm_front(i, xs_bufs, junk):
            n = pn_ctr[0]
            pn_ctr[0] += 1
            act(junk[n % 2], X[:, i, :], AF.Square, scale=1.0 / 32.0, accum=SS[:, i:i + 1])
            act(MS[:, i:i + 1], SS[:, i:i + 1], AF.Ln, bias=EPSB[:, 0:1])
            act(RSTD[:, i:i + 1], MS[:, i:i + 1], AF.Exp, scale=-0.5)
            xs = xs_bufs[n % 2]
            act(xs, X[:, i, :], AF.Copy, scale=RSTD[:, i:i + 1])
            return xs

        def prenorm_back(i, xs, gT, dst_of):
            b = nb()
            pb = ps[:, b, :].bitcast(BF16)
            for c in range(8):
                tr(pb[:, c * 128:(c + 1) * 128], xs[:, c * 128:(c + 1) * 128])
            tt("dve", dst_of(i), pb.rearrange("p (c t) -> p c t", t=128),
               gT.unsqueeze(2).to_broadcast([128, 8, 128]), ALU.mult)

        def prenorm(tiles, gT, dst_of, xs_bufs, junk):
            for i in tiles:
                xs = prenorm_front(i, xs_bufs, junk)
                prenorm_back(i, xs, gT, dst_of)

        def post_norm_residual(i, banks):
            for n, b in enumerate(banks):
                act(JUNK_F[n][:, 0:512], ps[:, b, :], AF.Square, scale=1.0 / 32.0, accum=SS2[:, 2 * i + n:2 * i + n + 1])
            tt("dve", MS2[:, i:i + 1], SS2[:, 2 * i:2 * i + 1], SS2[:, 2 * i + 1:2 * i + 2], ALU.add)
            act(MS2[:, i:i + 1], MS2[:, i:i + 1], AF.Ln, bias=EPSB[:, 0:1])
            act(RSTD2[:, i:i + 1], MS2[:, i:i + 1], AF.Exp, scale=-0.5)
            for n, b in enumerate(banks):
                tmp = TMP[n]
                tt("dve", tmp, ps[:, b, :], GBC[:, n * 512:(n + 1) * 512], ALU.mult)
                xv = X[:, i, n * 512:(n + 1) * 512]
                stt(xv, tmp, RSTD2[:, i:i + 1], xv, ALU.mult, ALU.add)

        for li in range(n_layers):
            l = first_layer + li
            last = (li == n_layers - 1)
            if li > 0:
                dma("sp", GTA, gtpre_d[l, 0], "p_gta")
            dma("sp", GTF, gtpre_d[l, 1], "p_gtf")
            dma("sp", CONVW.rearrange("p a b -> p (a b)"), convw_d[l], "p_convw")
            dma("sp", PSCALE, pscale_d[l], "p_pscale")
            dma("pool", POOLW.rearrange("p a b -> p (a b)"), poolw_d[l], "p_poolw")
            dma("sp", GBC, gpost_d[l, 0:1, :].partition_broadcast(128), "p_gbc")
            for j in range(4):
                dma("pool", WCP[:, 2 * j:2 * j + 2, :].rearrange("p a b -> p (a b)"), wcp_d[l, j], "w_ring")
            if li == 0:
                for i in range(4, NT):
                    dma("pool", X[:, i, :], x_d[i * 128:(i + 1) * 128, :], "x%d" % i)

            for m in range(2):
                memset("pool", UCV[m][:, 0:16], 0.0)
                memset("pool", UPL[m][:, 0:16], 0.0)
            ht_dst = lambda i: H[:, :, i * 128:(i + 1) * 128]
            prenorm(range(0, 4), GTA, ht_dst, XS_A, JUNK_A)
            for q in range(4):
                tok = slice(q * 512, (q + 1) * 512)
                par = q % 2

                def proj(j):
                    b = nb()
                    for kc in range(8):
                        mm(ps[:, b, :], WCP[:, kc, j * 128:(j + 1) * 128], H[:, kc, tok], kc == 0, kc == 7)
                    return ps[:, b, :]

                def conv_seg(m):
                    TA, TB, TC = CSC[par][m]
                    pc = proj(4 + m)
                    copy("act", TA[:, 16:528], pc)
                    ph = proj(0 + m)
                    tt("dve", UCV[m][:, 16:528], ph, TA[:, 16:528], ALU.mult)
                    pbb = proj(2 + m)
                    copy("act", TB[:, 16:528], pbb)
                    ts("dve", TC[:, 16:528], UCV[m][:, 14:526], CONVW[:, m, 0:1], None, ALU.mult)
                    stt(TC[:, 16:528], UCV[m][:, 15:527], CONVW[:, m, 1:2], TC[:, 16:528], ALU.mult, ALU.add)
                    stt(TC[:, 16:528], UCV[m][:, 16:528], CONVW[:, m, 2:3], TC[:, 16:528], ALU.mult, ALU.add)
                    tt("dve", CONCAT[:, 4 + m, tok], TB[:, 16:528], TC[:, 16:528], ALU.mult)
                    if q < 3:
                        copy("pool", UCV[m][:, 0:16], UCV[m][:, 512:528])

                def pool_front(m):
                    TA, TB = PSC[m]
                    pu = proj(6 + m)
                    copy("act", UPL[m][:, 16:528], pu)
                    up = UPL[m]
                    tt("pool", TA[:, 1:528], up[:, 1:528], up[:, 0:527], ALU.add)
                    tt("pool", TB[:, 3:528], TA[:, 3:528], TA[:, 1:526], ALU.add)
                    if m == 1:
                        tt("pool", TA[:, 7:528], TB[:, 7:528], TB[:, 3:524], ALU.add)
                        tt("pool", TB[:, 15:528], TA[:, 15:528], TA[:, 7:520], ALU.add)

                def pool_back(m):
                    TA, TB = PSC[m]
                    up = UPL[m]
                    wlo, whi = (2.0, 4.0) if m == 0 else (8.0, 16.0)
                    pl = PLB[par][m]
                    stt(pl[0:64, :], TA[0:64, 16:528], 1.0 / wlo, up[0:64, 16:528], ALU.mult, ALU.subtract)
                    stt(pl[64:128, :], TB[64:128, 16:528], 1.0 / whi, up[64:128, 16:528], ALU.mult, ALU.subtract)
                    if q == 0:
                        for lo, hi, src in ((0, 64, TA), (64, 128, TB)):
                            tt("dve", TD[lo:hi, 0:16], src[lo:hi, 16:32], RC[lo:hi, m, :], ALU.mult)
                            tt("dve", pl[lo:hi, 0:16], TD[lo:hi, 0:16], up[lo:hi, 16:32], ALU.subtract)
                    b = nb()
                    mm(ps[:, b, :], POOLW[:, m, :], pl, True, True)
                    ts("dve", CONCAT[:, 6 + m, tok], ps[:, b, :], PSCALE[:, m:m + 1], None, ALU.mult)
                    if q < 3:
                        copy("pool", up[:, 0:16], up[:, 512:528])

                segs = [lambda: (pool_front(0), pool_front(1)), lambda: conv_seg(0), lambda: conv_seg(1),
                        lambda: (pool_back(0), pool_back(1))]
                for si, seg in enumerate(segs):
                    nt_ = 4 * (q + 1) + si
                    if q == 0 and si == 0:
                        xs_pend = prenorm_front(nt_, XS_A, JUNK_A)
                    seg()
                    if q < 3:
                        prenorm_back(nt_, xs_pend, GTA, ht_dst)
                        if nt_ + 1 < NT:
                            xs_pend = prenorm_front(nt_ + 1, XS_A, JUNK_A)
            dump("hT", H.rearrange("p a b -> p (a b)"))

            dma("sp", KT[0][64:76, :], kaug_d[:, :], "c_kaug0")
            dma("sp", KT[1][64:76, :], kaug_d[:, :], "c_kaug1")

            for j in range(2):
                dma("pool", WV[:, 4 * j:4 * j + 4, :].rearrange("p a b -> p (a b)"), wv_d[l, j], "w_ring")
            dma("pool", WQK[0].rearrange("p a b c -> p (a b c)"), wqk_d[l, 0], "w_qk0")
            dma("pool", WQK[1].rearrange("p a b c -> p (a b c)"), wqk_d[l, 1], "w_qk1")
            memset("pool", V[:, :, :, 64:128], 1.0)
            for i in range(NT):
                b = nb()
                for kc in range(8):
                    mm(ps[:, b, :], H[:, kc, i * 128:(i + 1) * 128], WV[:, kc, :], kc == 0, kc == 7)
                src = ps[:, b, :].rearrange("p (a b c) -> p a b c", b=2, c=64)
                copy(alt(), V[:, i, :, 0:64], src[:, :, 0, :])
                copy(alt(), V[:, i, :, 128:192], src[:, :, 1, :])

            SB_, OB_, MB_ = (0, 1, 2, 5), (3, 4), (6, 7)
            sc = [0]
            oc = [0]
            mc = [0]
            ptc = [0]
            rdc = [0]

            def mb():
                b = MB_[mc[0] % 2]
                mc[0] += 1
                return b

            for p in range(4):
                wq = WQK[p % 2]
                if p >= 2:
                    dma("pool", wq.rearrange("p a b c -> p (a b c)"), wqk_d[l, p], "w_qk%d" % (p % 2))
                for hp in range(2):
                    dma("sp", QT[hp][64:76, :], qaug_d[2 * p + hp], "c_qaug%d" % hp)
                for tc in range(4):
                    tk = slice(tc * 512, (tc + 1) * 512)
                    b = mb()
                    for kc in range(8):
                        mm(ps[:, b, :], wq[:, 0, kc, :], H[:, kc, tk], kc == 0, kc == 7)
                    ts("dve", QT[0][0:64, tk], ps[0:64, b, :], 0.125, None, ALU.mult)
                    ts("dve", QT[1][0:64, tk], ps[64:128, b, :], 0.125, None, ALU.mult)
                    b = mb()
                    for kc in range(8):
                        mm(ps[:, b, :], wq[:, 1, kc, :], H[:, kc, tk], kc == 0, kc == 7)
                    copy("dve", KT[0][0:64, tk], ps[0:64, b, :])
                    copy("dve", KT[1][0:64, tk], ps[64:128, b, :])
                if p == 3:
                    for j in range(4):
                        dma("pool", WOUT[:, 2 * j:2 * j + 2, :].rearrange("p a b -> p (a b)"), wout_d[l, j], "w_ring")
                for hp in range(2):
                    kmf = KMF[hp]
                    T.op("dve", lambda e, kmf=kmf, kT=KT[hp]: e.tensor_reduce(
                        out=kmf[0:64, :], in_=kT[0:64, :].rearrange("p (n s) -> p n s", s=256), op=ALU.add, axis=AX.X),
                        reads=[KT[hp][0:64, :]], writes=[kmf[0:64, :]])
                    copy("dve", KMT[hp][0:64, :], kmf[0:64, :])

                def gate_part1(hp):
                    qT, kmt = QT[hp], KMT[hp]
                    gb = mb()
                    for j in range(8):
                        mm(ps[:, gb, j * 8:(j + 1) * 8], qT[0:64, (8 + j) * 128:(9 + j) * 128], kmt[0:64, :], True, True)
                    tt("dve", GM, ps[:, gb, 0:64], SBIAS, ALU.add)
                    for j in range(8):
                        T.op("dve", lambda e, j=j: e.max(out=TOP8[:, j * 8:(j + 1) * 8], in_=GM[:, j * 8:(j + 1) * 8]),
                             reads=[GM[:, j * 8:(j + 1) * 8]], writes=[TOP8[:, j * 8:(j + 1) * 8]])
                    thr = TOP8.rearrange("p (a b) -> p a b", b=8)[:, :, 3:4].to_broadcast([128, 8, 8])
                    T.op("dve", lambda e, thr=thr: e.tensor_tensor(
                        out=SELB.rearrange("p (a b) -> p a b", b=8), in0=GM.rearrange("p (a b) -> p a b", b=8),
                        in1=thr, op=ALU.is_ge), reads=[GM, TOP8], writes=[SELB])
                    ts("dve", BIASB[:, 64:128], SELB, -1.0, -NEG, ALU.add, ALU.mult)

                def gate_part2(hp):
                    qT = QT[hp]
                    for r in range(2):
                        b = mb()
                        for jj in range(4):
                            j = 4 * r + jj
                            mm(ps[0:72, b, jj * 128:(jj + 1) * 128], BIASB[:, j * 8:j * 8 + 72], IDENT, True, True)
                        copy("act" if r == 0 else "dve", qT[64:72, 1024 + 512 * r:1536 + 512 * r], ps[64:72, b, :])

                items = []
                for hp in range(2):
                    for c in range(4):
                        steps = []
                        for kt in range(4 * c + 4):
                            kb = kt // 2
                            if kb < 2 * c:
                                steps.append((kt, 0, 512, None))
                            elif kb == 2 * c:
                                steps.append((kt, 0, 512, 0) if kt == 4 * c else (kt, 128, 512, 128))
                            else:
                                steps.append((kt, 256, 512, 256) if kt == 4 * c + 2 else (kt, 384, 512, 384))
                        ob = OB_[oc[0] % 2]
                        oc[0] += 1
                        for si, st_ in enumerate(steps):
                            items.append((hp, c, ob, st_, si == 0, si == len(steps) - 1))
                LA = 3
                pts = {}
                for s_ in range(len(items) + LA):
                    if s_ < len(items):
                        hp, c, ob, (kt, q0, q1, tri), first, lastst = items[s_]
                        qT, kT = QT[hp], KT[hp]
                        Sb = ps[:, SB_[sc[0] % 4], :]
                        sc[0] += 1
                        mm(Sb[:, q0:q1], kT[0:76, kt * 128:(kt + 1) * 128], qT[0:76, c * 512 + q0:c * 512 + q1],
                           True, tri is None)
                        if tri is not None:
                            mm(Sb[:, tri:tri + 128], IDENT, TRI, False, True)
                        pt = PT[ptc[0] % 4]
                        ptc[0] += 1
                        act(pt[:, q0:q1], Sb[:, q0:q1], AF.Exp)
                        pts[s_] = pt
                    if s_ - LA >= 0:
                        hp, c, ob, (kt, q0, q1, tri), first, lastst = items[s_ - LA]
                        hd = 2 * p + hp
                        O = ps[:, ob, :]
                        pt = pts.pop(s_ - LA)
                        mm(O[:, q0:q1], vaug(kt, hd), pt[:, q0:q1], first, lastst)
                        if lastst:
                            rd = RDEN[rdc[0] % 2]
                            rdc[0] += 1
                            nlo, dlo = (0, 64) if hp == 0 else (64, 0)
                            if c % 2 == 0:
                                T.op("dve", lambda e, rd=rd, O=O, nlo=nlo, dlo=dlo: e.reciprocal(
                                    out=rd[nlo:nlo + 64, :], in_=O[dlo:dlo + 64, :]),
                                    reads=[O[dlo:dlo + 64, :]], writes=[rd[nlo:nlo + 64, :]])
                            else:
                                act(rd[nlo:nlo + 64, :], O[dlo:dlo + 64, :], AF.Ln)
                                act(rd[nlo:nlo + 64, :], rd[nlo:nlo + 64, :], AF.Exp, scale=-1.0)
                            tt("dve", CONCAT[nlo:nlo + 64, p, c * 512:(c + 1) * 512], O[nlo:nlo + 64, :], rd[nlo:nlo + 64, :], ALU.mult)
                            if c == 0:
                                gate_part1(hp)
                            if c == 1:
                                gate_part2(hp)
            dump("concat", CONCAT.rearrange("p a b -> p (a b)"))

            for j in range(11):
                dma("pool", WD[:, 2 * j:2 * j + 2, :].rearrange("p a b -> p (a b)"), wd_d[l, j], "w_wd")

            hf_dst = lambda i: HF[:, :, (i % 8) * 128:(i % 8 + 1) * 128]
            bank_of = {}
            xs_of = {}
            for i in range(NT + 4):
                if i < NT:
                    banks = (nb(), nb())
                    bank_of[i] = banks
                    for n, b in enumerate(banks):
                        for kc in range(8):
                            mm(ps[:, b, :], CONCAT[:, kc, i * 128:(i + 1) * 128], WOUT[:, kc, n * 512:(n + 1) * 512], kc == 0, kc == 7)
                j3 = i - 3
                if 0 <= j3 < 8:
                    bt = nb()
                    pbt = ps[:, bt, :].bitcast(BF16)
                    for c in range(8):
                        tr(pbt[:, c * 128:(c + 1) * 128], xs_of[j3][:, c * 128:(c + 1) * 128])
                j1 = i - 1
                if 0 <= j1 < NT:
                    for n, b in enumerate(bank_of[j1]):
                        tmp = TMP[n]
                        tt("dve", tmp, ps[:, b, :], GBC[:, n * 512:(n + 1) * 512], ALU.mult)
                        xv = X[:, j1, n * 512:(n + 1) * 512]
                        stt(xv, tmp, RSTD2[:, j1:j1 + 1], xv, ALU.mult, ALU.add)
                j2 = i - 2
                if 0 <= j2 < 8:
                    n_ = pn_ctr[0]
                    pn_ctr[0] += 1
                    xs = XS_F[n_ % 2]
                    act(xs, X[:, j2, :], AF.Copy, scale=RSTD[:, j2:j2 + 1])
                    xs_of[j2] = xs
                if i < NT:
                    for n, b in enumerate(bank_of[i]):
                        act(JUNK_F[n][:, 0:512], ps[:, b, :], AF.Square, scale=1.0 / 32.0, accum=SS2[:, 2 * i + n:2 * i + n + 1])
                    tt("dve", MS2[:, i:i + 1], SS2[:, 2 * i:2 * i + 1], SS2[:, 2 * i + 1:2 * i + 2], ALU.add)
                    act(MS2[:, i:i + 1], MS2[:, i:i + 1], AF.Ln, bias=EPSB[:, 0:1])
                    act(RSTD2[:, i:i + 1], MS2[:, i:i + 1], AF.Exp, scale=-0.5)
                if 0 <= j1 < 8:
                    act(JUNK_F[j1 % 2], X[:, j1, :], AF.Square, scale=1.0 / 32.0, accum=SS[:, j1:j1 + 1])
                    act(MS[:, j1:j1 + 1], SS[:, j1:j1 + 1], AF.Ln, bias=EPSB[:, 0:1])
                    act(RSTD[:, j1:j1 + 1], MS[:, j1:j1 + 1], AF.Exp, scale=-0.5)
                if 0 <= j3 < 8:
                    tt("dve", hf_dst(j3), pbt.rearrange("p (c t) -> p c t", t=128),
                       GTF.unsqueeze(2).to_broadcast([128, 8, 128]), ALU.mult)
            dump("xmid", mem[:, X_OFF:X_OFF + 65536].bitcast(F32))
            dma("sp", GBC, gpost_d[l, 1:2, :].partition_broadcast(128), "p_gbc")

            gu_next = [0]

            def gu_prefetch(upto):
                while gu_next[0] <= upto:
                    g = gu_next[0]
                    dma("pool", GU[g % NS].rearrange("p a b c -> p (a b c)"), wgu_d[l, g % NF], "w_gu%d" % (g % NS))
                    gu_next[0] += 1

            for hh in range(2):
                sgc = 0
                for f in range(NF):
                    g = hh * NF + f
                    gu_prefetch(min(g + NS - 1, 2 * NF - 1))
                    slot = GU[g % NS]
                    for t2 in range(2):
                        tk = slice(t2 * 512, (t2 + 1) * 512)
                        bg, bu = nb(), nb()
                        for kc in range(8):
                            mm(ps[:, bg, :], slot[:, 0, kc, :], HF[:, kc, tk], kc == 0, kc == 7)
                        for kc in range(8):
                            mm(ps[:, bu, :], slot[:, 1, kc, :], HF[:, kc, tk], kc == 0, kc == 7)
                        sg = SG[sgc % 2]
                        sgc += 1
                        act(sg, ps[:, bg, :], AF.Silu)
                        tt("dve", ACTT[:, f, tk], ps[:, bu, :], sg, ALU.mult)
                for i8 in range(8):
                    gi = hh * 8 + i8
                    xsb = prenorm_front(8 + i8, XS_F, JUNK_F) if hh == 0 else None
                    banks = (nb(), nb())
                    for n, b in enumerate(banks):
                        for f in range(NF):
                            mm(ps[:, b, :], ACTT[:, f, i8 * 128:(i8 + 1) * 128], WD[:, f, n * 512:(n + 1) * 512], f == 0, f == NF - 1)
                    if hh == 0:
                        prenorm_back(8 + i8, xsb, GTF, hf_dst)
                    post_norm_residual(gi, banks)
                    if last:
                        out_tok = dma("sp", y_d[gi * 128:(gi + 1) * 128, :], X[:, gi, :], "out")

        T.wait_all("sp", [out_tok])
        T.emit()
        nc._trk_stats = T.stats
    return nc


def _consts():
    bf = ml_dtypes.bfloat16
    ident = np.eye(128, dtype=np.float32).astype(bf)
    s_idx = np.arange(128)[:, None]
    t_idx = np.arange(128)[None, :]
    tri = np.where(s_idx <= t_idx, 0.0, NEG).astype(np.float32).astype(bf)
    pos = np.arange(S)
    blk = pos // 256
    rem = pos % 256
    kaug = np.zeros((12, S), np.float32)
    for j in range(8):
        kaug[j] = (blk == j)
    kaug[8] = 1.0
    kaug[9] = 1.0
    kaug[10] = 256.0 * blk
    kaug[11] = rem
    qaug = np.zeros((8, 12, S), np.float32)
    for h in range(8):
        slope = 2.0 ** (-(h + 1))
        qaug[h, 8] = -slope * 256.0 * blk
        qaug[h, 9] = -slope * rem
        qaug[h, 10] = slope
        qaug[h, 11] = slope
    sbias = np.zeros((8, 8), np.float32)
    for j in range(8):
        cur = (8 + j) // 2
        for n in range(8):
            sbias[j, n] = 0.0 if n < cur else (1e30 if n == cur else -1e30)
    sbias = np.broadcast_to(sbias.reshape(1, 64), (128, 64)).copy()
    rc = np.zeros((128, 2, 16), np.float32)
    wins = (2, 4, 8, 16)
    for pp in range(128):
        for m in range(2):
            w = wins[2 * m + pp // 64]
            rc[pp, m] = 1.0 / np.minimum(np.arange(16) + 1, w)
    return dict(ident=ident, tri=tri, kaug=kaug.astype(bf), qaug=qaug.astype(bf), sbias=sbias,
                rc=rc.reshape(128, 32))


def _prep_weights(w_in, w_out, conv_w, pool_w, pool_scale, g_pre_mix, g_post_mix, g_pre_ffn, g_post_ffn,
                  w_gate, w_up, w_down):
    L = DEPTH
    f32 = np.float32
    w_in = np.asarray(w_in, f32)
    win_r = w_in.reshape(L, 8, 128, 2560).transpose(0, 2, 1, 3)
    wcp = win_r[:, :, :, 1536:2560].reshape(L, 128, 4, 2, 1024).transpose(0, 2, 1, 3, 4).reshape(L, 4, 128, 2048)
    wv = win_r[:, :, :, 1024:1536].reshape(L, 128, 2, 4, 512).transpose(0, 2, 1, 3, 4).reshape(L, 2, 128, 2048)
    wq = win_r[:, :, :, 0:512].reshape(L, 128, 8, 4, 128)
    wk = win_r[:, :, :, 512:1024].reshape(L, 128, 8, 4, 128)
    wqk = np.stack([wq, wk], axis=2)
    wqk = wqk.transpose(0, 4, 1, 2, 3, 5).reshape(L, 4, 128, 2048)
    wout = np.asarray(w_out, f32).reshape(L, 8, 128, 1024).transpose(0, 2, 1, 3)
    wout = wout.reshape(L, 128, 4, 2, 1024).transpose(0, 2, 1, 3, 4).reshape(L, 4, 128, 2048)
    wg = np.asarray(w_gate, f32).reshape(L, 8, 128, NF, 128)
    wu = np.asarray(w_up, f32).reshape(L, 8, 128, NF, 128)
    wgu = np.stack([wg, wu], axis=1)
    wgu = wgu.transpose(0, 4, 3, 1, 2, 5).reshape(L, NF, 128, 2048)
    wd = np.asarray(w_down, f32).reshape(L, 11, 2, 128, 1024).transpose(0, 1, 3, 2, 4).reshape(L, 11, 128, 2048)
    gpost = np.stack([np.asarray(g_post_mix, f32), np.asarray(g_post_ffn, f32)], axis=1)
    gtpre = np.stack([np.asarray(g_pre_mix, f32).reshape(L, 8, 128).transpose(0, 2, 1),
                      np.asarray(g_pre_ffn, f32).reshape(L, 8, 128).transpose(0, 2, 1)], axis=1)
    convw = np.asarray(conv_w, f32).transpose(0, 2, 1).reshape(L, 2, 128, 3).transpose(0, 2, 1, 3).reshape(L, 128, 6)
    pscale = np.asarray(pool_scale, f32).reshape(L, 2, 128).transpose(0, 2, 1)
    pw = np.asarray(pool_w, f32)
    poolw = np.zeros((L, 128, 2, 128), f32)
    for m in range(2):
        poolw[:, 0:64, m, 0:64] = pw[:, 2 * m]
        poolw[:, 64:128, m, 64:128] = pw[:, 2 * m + 1]
    poolw = poolw.reshape(L, 128, 256)
    c = np.ascontiguousarray
    return dict(wcp=c(wcp), wv=c(wv), wqk=c(wqk), wout=c(wout), wgu=c(wgu), wd=c(wd), gpost=c(gpost),
                gtpre=c(gtpre), convw=c(convw), pscale=c(pscale), poolw=c(poolw))


_NC_CACHE = {}


def kernel(x, w_in, w_out, conv_w, pool_w, pool_scale, g_pre_mix, g_post_mix, g_pre_ffn, g_post_ffn,
           w_gate, w_up, w_down):
    x = np.asarray(x, np.float32)
    shared = _prep_weights(w_in, w_out, conv_w, pool_w, pool_scale, g_pre_mix, g_post_mix, g_pre_ffn,
                           g_post_ffn, w_gate, w_up, w_down)
    shared.update(_consts())
    if "nc" not in _NC_CACHE:
        _NC_CACHE["nc"] = build_nc()
    nc = _NC_CACHE["nc"]
    in_maps = []
    for b in range(8):
        m = dict(shared)
        m["x"] = np.ascontiguousarray(x[b])
        in_maps.append(m)
    res = run_bass_kernel_spmd(nc, in_maps, core_ids=list(range(8)))
    return np.stack([np.asarray(r["y"], np.float32) for r in res.results], axis=0)
```

```python
import contextlib
import numpy as np
import ml_dtypes
import concourse.bass as bass
import concourse.mybir as mybir
from concourse.bass_utils import run_bass_kernel_spmd

F32 = mybir.dt.float32
BF16 = mybir.dt.bfloat16
U8 = mybir.dt.uint8
FP8 = mybir.dt.float8e4
AF = mybir.ActivationFunctionType
ALU = mybir.AluOpType
AX = mybir.AxisListType

D = 1024
S = 2048
NT = 16
DEPTH = 2
DFF = 2816
NF = 22
EPS = 1e-6
NEG = -30000.0
ENGS = ("pe", "act", "dve", "pool", "sp")
STRICT_SAME_ENGINE = True


_ES = {"float8e4": 1, "float32": 4, "bfloat16": 2, "uint8": 1, "int32": 4, "uint32": 4, "float16": 2, "uint16": 2}


class Tracker:
    def __init__(self, nc):
        self.nc = nc
        self.ops = []
        self.idx = {}
        self.known = {e: {} for e in ENGS}
        self.clock = {}
        self.sb = {}
        self.ps = {}
        self.G = 512
        self.dma_keys = set()

    @staticmethod
    def _box(ap):
        t = ap.tensor
        tn = type(t).__name__
        if tn.startswith("SB"):
            space = "sb"
        elif tn.startswith("PS"):
            space = "ps"
        else:
            return None
        es = _ES[str(ap.dtype).split(".")[-1]]
        dims = list(ap.ap)
        pstride, pcount = dims[0]
        off = ap.offset
        p0 = off // pstride if pstride else 0
        f0 = off - p0 * pstride
        lo = hi = f0
        for st, cnt in dims[1:]:
            if st >= 0:
                hi += st * (cnt - 1)
            else:
                lo += st * (cnt - 1)
        return (space, p0, p0 + pcount, lo * es, (hi + 1) * es)

    def _conflicts(self, box, mode):
        space, p0, p1, b0, b1 = box
        out = []
        if space == "ps":
            for bank in range(b0 // 2048, (b1 - 1) // 2048 + 1):
                out.extend(self.ps.get(bank, ()))
            return out
        seen = set()
        for g in range(b0 // self.G, (b1 - 1) // self.G + 1):
            for r in self.sb.get(g, ()):
                if id(r) in seen:
                    continue
                rp0, rp1, rb0, rb1, rmode = r[0], r[1], r[2], r[3], r[4]
                if rb0 < b1 and b0 < rb1 and rp0 < p1 and p0 < rp1 and (mode == "w" or rmode == "w"):
                    seen.add(id(r))
                    out.append(r)
        return out

    def _register(self, box, mode, key, idx, eng):
        space, p0, p1, b0, b1 = box
        if space == "ps":
            rec = (0, 128, 0, 0, mode, key, idx, eng)
            for bank in range(b0 // 2048, (b1 - 1) // 2048 + 1):
                self.ps[bank] = [rec]
            return
        rec = (p0, p1, b0, b1, mode, key, idx, eng)
        G = self.G
        for g in range(b0 // G, (b1 - 1) // G + 1):
            lst = self.sb.get(g)
            if lst is None:
                self.sb[g] = [rec]
                continue
            new = []
            for r in lst:
                rp0, rp1, rb0, rb1, rmode, rkey = r[0], r[1], r[2], r[3], r[4], r[5]
                gb0 = max(rb0, g * G)
                gb1 = min(rb1, (g + 1) * G)
                covered = (p0 <= rp0 and rp1 <= p1 and b0 <= gb0 and gb1 <= b1)
                if covered and (mode == "w" or (rmode == "r" and rkey == key)):
                    continue
                new.append(r)
            new.append(rec)
            self.sb[g] = new

    def op(self, eng, fn, reads=(), writes=(), dma=None):
        key = dma if dma is not None else eng
        if dma is not None:
            self.dma_keys.add(dma)
        idx = self.idx.get(key, 0) + 1
        self.idx[key] = idx
        known = self.known[eng]
        need = {}
        rboxes = [b for b in (self._box(a) for a in reads) if b is not None]
        wboxes = [b for b in (self._box(a) for a in writes) if b is not None]
        for boxes, mode in ((rboxes, "r"), (wboxes, "w")):
            for box in boxes:
                for r in self._conflicts(box, mode):
                    rmode, rkey, ridx = r[4], r[5], r[6]
                    if rkey == key:
                        if dma is not None:
                            pass
                        elif eng == "pe":
                            continue
                        elif STRICT_SAME_ENGINE or (rmode == "w" and mode == "r"):
                            pass
                        else:
                            continue
                    if rkey in self.dma_keys:
                        ridx = self.idx[rkey] if rkey != key else idx - 1
                    if need.get(rkey, 0) < ridx:
                        need[rkey] = ridx
        waits = []
        for k, v in need.items():
            if v <= 0 or known.get(k, 0) >= v:
                continue
            waits.append((k, v))
            known[k] = v
            ck = self.clock.get((k, v))
            if ck:
                for kk, vv in ck.items():
                    if known.get(kk, 0) < vv:
                        known[kk] = vv
        snap = dict(known)
        snap[key] = idx
        self.clock[(key, idx)] = snap
        for box in rboxes:
            self._register(box, "r", key, idx, eng)
        for box in wboxes:
            self._register(box, "w", key, idx, eng)
        self.ops.append([eng, key, idx, fn, waits, dma is not None])
        return (key, idx)

    def wait_all(self, eng, tokens):
        self.ops.append([eng, None, None, None, list(tokens), False])

    def emit(self):
        nc = self.nc
        sig = {}
        for eng, key, idx, fn, waits, is_dma in self.ops:
            for k, v in waits:
                sig.setdefault(k, set()).add(v)
        dma_keys = self.dma_keys
        cnt_of = {}
        for k, s in sig.items():
            if k in dma_keys:
                continue
            for n, v in enumerate(sorted(s)):
                cnt_of[(k, v)] = n + 1
        keys = sorted(set(sig.keys()) | set(dma_keys))
        sems = {}
        with contextlib.ExitStack() as st:
            for k in keys:
                sems[k] = st.enter_context(nc.semaphore("s_" + k))
            block = st.enter_context(nc.Block())
            per_eng = {e: [] for e in ENGS}
            for o in self.ops:
                per_eng[o[0]].append(o)

            def body(e, eng):
                for _, key, idx, fn, waits, is_dma in per_eng[eng]:
                    for k, v in waits:
                        val = 16 * v if k in dma_keys else cnt_of[(k, v)]
                        e.wait_ge(sems[k], val)
                    if fn is None:
                        continue
                    ins = fn(e)
                    if is_dma:
                        ins.then_inc(sems[key], 16)
                    elif (key, idx) in cnt_of:
                        ins.then_inc(sems[key], 1)

            @block.tensor
            def _(e):
                body(e, "pe")

            @block.scalar
            def _(e):
                body(e, "act")

            @block.vector
            def _(e):
                body(e, "dve")

            @block.gpsimd
            def _(e):
                body(e, "pool")

            @block.sync
            def _(e):
                body(e, "sp")
        self.stats = {e: len(v) for e, v in per_eng.items()}
        self.stats["waits"] = sum(len(o[4]) for o in self.ops)
        self.stats["sems"] = len(keys)


X_OFF = 0
H_OFF = 65536
R2_OFF = 98304
RING_OFF = 188416
C_OFF = 204800
MEM_BYTES = 212800

CONCAT_OFF = R2_OFF + 0
V_OFF = R2_OFF + 32768
B1_OFF = R2_OFF + 32768
QK_OFF = R2_OFF + 57344
PT_OFF = R2_OFF + 73728
RDEN_OFF = R2_OFF + 77824
MISC_OFF = R2_OFF + 81920
ACTT_OFF = R2_OFF + 0
WD_OFF = R2_OFF + 45056
HU_OFF = H_OFF + 16384
GBC_OFF = C_OFF + 0
IDENT_OFF = C_OFF + 4096
TRI_OFF = C_OFF + 4352
SBIAS_OFF = C_OFF + 4608
GM_OFF = C_OFF + 4864
TOP8_OFF = C_OFF + 5120
SELB_OFF = C_OFF + 5376
BIASB_OFF = C_OFF + 5632
KMT_OFF = C_OFF + 5888
STAT_OFF = C_OFF + 5920
CONVW_OFF = C_OFF + 6368
PSCALE_OFF = C_OFF + 6392
RC_OFF = C_OFF + 6400
POOLW_OFF = C_OFF + 6528
POWC_OFF = C_OFF + 7040
GTA_OFF = C_OFF + 7044
GTF_OFF = C_OFF + 7076
KMF_OFF = C_OFF + 7108


def build_nc(n_layers=DEPTH, first_layer=0, dbg=()):
    nc = bass.Bass("TRN2", target_bir_lowering=False)
    L = DEPTH

    def din(name, shape, dt=F32):
        return nc.dram_tensor(name, list(shape), dt, kind="ExternalInput").ap()

    x_d = din("x", [S, D])
    wcp_d = din("wcp", [L, 4, 128, 2048])
    wv_d = din("wv", [L, 2, 128, 2048])
    wqk_d = din("wqk", [L, 4, 128, 2048])
    wout_d = din("wout", [L, 4, 128, 2048])
    wgu_d = din("wgu", [L, NF, 128, 2048])
    wd_d = din("wd", [L, 11, 128, 2048])
    gpost_d = din("gpost", [L, 2, D])
    gtpre_d = din("gtpre", [L, 2, 128, 8])
    convw_d = din("convw", [L, 128, 6])
    pscale_d = din("pscale", [L, 128, 2])
    poolw_d = din("poolw", [L, 128, 256])
    ident_d = din("ident", [128, 128], BF16)
    tri_d = din("tri", [128, 128], BF16)
    kaug_d = din("kaug", [12, S], BF16)
    qaug_d = din("qaug", [8, 12, S], BF16)
    sbias_d = din("sbias", [128, 64])
    rc_d = din("rc", [128, 32])
    y_d = nc.dram_tensor("y", [S, D], F32, kind="ExternalOutput").ap()
    dbg_out = {}
    for name, shape, dt in dbg:
        dbg_out[name] = nc.dram_tensor("dbg_" + name, list(shape), dt, kind="ExternalOutput").ap()

    with contextlib.ExitStack() as st:
        mem = st.enter_context(nc.sbuf_tensor("mem", [128, MEM_BYTES], U8))
        ps = st.enter_context(nc.psum_tensor("ps", [128, 8, 512], F32))
        T = Tracker(nc)
        memb = mem.bitcast(BF16)

        def sbv(off, shape, dt):
            es = 4 if dt == F32 else 2
            n = int(np.prod(shape))
            v = mem[:, off:off + n * es].bitcast(dt)
            if len(shape) == 2:
                v = v.rearrange("p (a b) -> p a b", b=shape[1])
            elif len(shape) == 3:
                v = v.rearrange("p (a b c) -> p a b c", b=shape[1], c=shape[2])
            return v

        X = sbv(X_OFF, (NT, D), F32)
        H = sbv(H_OFF, (8, S), BF16)
        HF = sbv(H_OFF, (8, 1024), BF16)
        CONCAT = sbv(CONCAT_OFF, (8, S), BF16)
        V = sbv(V_OFF, (NT, 4, 192), BF16)
        QT = [sbv(QK_OFF + 0, (S,), BF16), sbv(QK_OFF + 4096, (S,), BF16)]
        KT = [sbv(QK_OFF + 8192, (S,), BF16), sbv(QK_OFF + 12288, (S,), BF16)]
        QT2 = [sbv(RING_OFF + 0, (S,), BF16), sbv(RING_OFF + 4096, (S,), BF16)]
        KT2 = [sbv(MISC_OFF + 0, (S,), BF16), sbv(MISC_OFF + 4096, (S,), BF16)]
        QKSETS = [(QT2, KT2), (QT, KT)]
        TS_ = 2112
        UCV = [sbv(B1_OFF + m * TS_, (528,), F32) for m in range(2)]
        UPL = [sbv(B1_OFF + (2 + m) * TS_, (528,), F32) for m in range(2)]
        CSC = [[[sbv(B1_OFF + (4 + (par * 2 + m) * 3 + k) * TS_, (528,), F32) for k in range(3)]
                for m in range(2)] for par in range(2)]
        PSC = [[sbv(B1_OFF + (16 + m * 2 + k) * TS_, (528,), F32) for k in range(2)] for m in range(2)]
        PLB = [[sbv(B1_OFF + 20 * TS_ + (par * 2 + m) * 1024, (512,), BF16) for m in range(2)] for par in range(2)]
        TD = sbv(MISC_OFF + 6144, (16,), F32)
        PT = [sbv(PT_OFF + 1024 * j, (512,), BF16) for j in range(4)]
        RDEN = [sbv(RDEN_OFF + 2048 * j, (512,), F32) for j in range(2)]
        XS_A = [sbv(MISC_OFF + 2048 * j, (D,), BF16) for j in range(2)]
        JUNK_A = [mem[:, MISC_OFF + 4096 + 1024 * j:MISC_OFF + 4096 + 1024 * (j + 1)] for j in range(2)]
        ACTT = sbv(ACTT_OFF, (NF, 1024), BF16)
        WD = sbv(WD_OFF, (NF, 1024), BF16)
        XS_F = [sbv(HU_OFF + 2048 * j, (D,), BF16) for j in range(2)]
        JUNK_F = [mem[:, HU_OFF + 4096 + 1024 * j:HU_OFF + 4096 + 1024 * (j + 1)] for j in range(2)]
        SG = [sbv(HU_OFF + 6144 + 1024 * j, (512,), BF16) for j in range(2)]
        TMP = [sbv(HU_OFF + 8192 + 2048 * j, (512,), F32) for j in range(2)]
        WCP = sbv(RING_OFF, (8, 1024), BF16)
        WV = sbv(RING_OFF, (8, 512), BF16)
        WQK = [sbv(RING_OFF + 8192 + 4096 * j, (2, 8, 128), BF16) for j in range(2)]
        WOUT = sbv(RING_OFF, (8, 1024), BF16)
        NS = 4
        GU = [sbv(RING_OFF + 4096 * j, (2, 8, 128), BF16) for j in range(NS)]
        GBC = sbv(GBC_OFF, (D,), F32)
        IDENT = sbv(IDENT_OFF, (128,), BF16)
        TRI = sbv(TRI_OFF, (128,), BF16)
        SBIAS = sbv(SBIAS_OFF, (64,), F32)
        GM = sbv(GM_OFF, (64,), F32)
        TOP8 = sbv(TOP8_OFF, (64,), F32)
        SELB = sbv(SELB_OFF, (64,), F32)
        BIASB = sbv(BIASB_OFF, (128,), BF16)
        KMT = [sbv(KMT_OFF + 16 * j, (8,), BF16) for j in range(2)]
        KMF = [sbv(KMF_OFF + 32 * j, (8,), F32) for j in range(2)]
        STAT = sbv(STAT_OFF, (112,), F32)
        SS, MS, RSTD = STAT[:, 0:16], STAT[:, 16:32], STAT[:, 32:48]
        SS2, MS2, RSTD2 = STAT[:, 48:80], STAT[:, 80:96], STAT[:, 96:112]
        CONVW = sbv(CONVW_OFF, (2, 3), F32)
        PSCALE = sbv(PSCALE_OFF, (2,), F32)
        RC = sbv(RC_OFF, (2, 16), F32)
        POOLW = sbv(POOLW_OFF, (2, 128), BF16)
        EPSB = sbv(POWC_OFF, (1,), F32)
        GTA = sbv(GTA_OFF, (8,), F32)
        GTF = sbv(GTF_OFF, (8,), F32)

        def vaug(i, h):
            lo = 0 if h % 2 == 0 else 64
            return V[:, i, h // 2, lo:lo + 128]

        def isap(v):
            return hasattr(v, "tensor") and hasattr(v, "ap")

        def mm(out, lhsT, rhs, start, stop):
            T.op("pe", lambda e: e.matmul(out, lhsT=lhsT, rhs=rhs, start=start, stop=stop,
                                          skip_group_check=True),
                 reads=[lhsT, rhs], writes=[out])

        def tr(out, in_):
            T.op("pe", lambda e: e.transpose(out=out, in_=in_, identity=IDENT),
                 reads=[in_, IDENT], writes=[out])

        def act(out, in_, func, scale=None, accum=None, bias=None):
            kw = {}
            if bias is not None:
                kw["bias"] = bias
            rd = [in_]
            wr = [out]
            if scale is not None:
                kw["scale"] = scale
                if isap(scale):
                    rd.append(scale)
            if accum is not None:
                kw["accum_out"] = accum
                wr.append(accum)
            T.op("act", lambda e: e.activation(out=out, in_=in_, func=func, **kw), reads=rd, writes=wr)

        def tt(eng, out, in0, in1, op):
            T.op(eng, lambda e: e.tensor_tensor(out=out, in0=in0, in1=in1, op=op),
                 reads=[in0, in1], writes=[out])

        def ts(eng, out, in0, s1, s2, op0, op1=None):
            rd = [in0] + [s for s in (s1, s2) if isap(s)]
            if op1 is None:
                T.op(eng, lambda e: e.tensor_scalar(out=out, in0=in0, scalar1=s1, scalar2=None, op0=op0),
                     reads=rd, writes=[out])
            else:
                T.op(eng, lambda e: e.tensor_scalar(out=out, in0=in0, scalar1=s1, scalar2=s2, op0=op0, op1=op1),
                     reads=rd, writes=[out])

        def stt(out, in0, scalar, in1, op0, op1):
            rd = [in0, in1] + ([scalar] if isap(scalar) else [])
            T.op("dve", lambda e: e.scalar_tensor_tensor(out=out, in0=in0, scalar=scalar, in1=in1, op0=op0, op1=op1),
                 reads=rd, writes=[out])

        def copy(eng, out, in_):
            if eng == "act":
                act(out, in_, AF.Copy)
            else:
                T.op(eng, lambda e: e.tensor_copy(out=out, in_=in_), reads=[in_], writes=[out])

        def memset(eng, out, val):
            T.op(eng, lambda e: e.memset(out, val), writes=[out])

        def dma(q, out, in_, stream):
            return T.op(q, lambda e: e.dma_start(out=out, in_=in_), reads=[in_], writes=[out], dma=stream)

        bank_ctr = [0]

        def nb():
            b = bank_ctr[0] % 8
            bank_ctr[0] += 1
            return b

        alt_ctr = [0]

        def alt():
            alt_ctr[0] += 1
            return "act" if alt_ctr[0] % 2 else "dve"

        def dump(name, src):
            if name in dbg_out:
                dma("sp", dbg_out[name], src, "dbg_" + name)

        dma("sp", IDENT, ident_d[:, :], "c_ident")
        dma("sp", GTA, gtpre_d[first_layer, 0], "p_gta")
        for i in range(4):
            dma("sp", X[:, i, :], x_d[i * 128:(i + 1) * 128, :], "x%d" % i)
        dma("sp", TRI, tri_d[:, :], "c_tri")
        dma("sp", SBIAS, sbias_d[:, :], "c_sbias")
        dma("sp", RC.rearrange("p a b -> p (a b)"), rc_d[:, :], "c_rc")
        memset("dve", BIASB[:, 0:64], 0.0)
        memset("dve", EPSB, EPS)

        pn_ctr = [0]

        def prenorm_front(i, xs_bufs, junk):
            n = pn_ctr[0]
            pn_ctr[0] += 1
            act(junk[n % 2], X[:, i, :], AF.Square, scale=1.0 / 32.0, accum=SS[:, i:i + 1])
            act(MS[:, i:i + 1], SS[:, i:i + 1], AF.Ln, bias=EPSB[:, 0:1])
            act(RSTD[:, i:i + 1], MS[:, i:i + 1], AF.Exp, scale=-0.5)
            xs = xs_bufs[n % 2]
            act(xs, X[:, i, :], AF.Copy, scale=RSTD[:, i:i + 1])
            return xs

        def prenorm_back(i, xs, gT, dst_of):
            b = nb()
            pb = ps[:, b, :].bitcast(BF16)
            for c in range(8):
                tr(pb[:, c * 128:(c + 1) * 128], xs[:, c * 128:(c + 1) * 128])
            tt("dve", dst_of(i), pb.rearrange("p (c t) -> p c t", t=128),
               gT.unsqueeze(2).to_broadcast([128, 8, 128]), ALU.mult)

        def prenorm(tiles, gT, dst_of, xs_bufs, junk):
            for i in tiles:
                xs = prenorm_front(i, xs_bufs, junk)
                prenorm_back(i, xs, gT, dst_of)

        def post_norm_residual(i, banks):
            for n, b in enumerate(banks):
                act(JUNK_F[n][:, 0:512], ps[:, b, :], AF.Square, scale=1.0 / 32.0, accum=SS2[:, 2 * i + n:2 * i + n + 1])
            tt("dve", MS2[:, i:i + 1], SS2[:, 2 * i:2 * i + 1], SS2[:, 2 * i + 1:2 * i + 2], ALU.add)
            act(MS2[:, i:i + 1], MS2[:, i:i + 1], AF.Ln, bias=EPSB[:, 0:1])
            act(RSTD2[:, i:i + 1], MS2[:, i:i + 1], AF.Exp, scale=-0.5)
            for n, b in enumerate(banks):
                tmp = TMP[n]
                tt("dve", tmp, ps[:, b, :], GBC[:, n * 512:(n + 1) * 512], ALU.mult)
                xv = X[:, i, n * 512:(n + 1) * 512]
                stt(xv, tmp, RSTD2[:, i:i + 1], xv, ALU.mult, ALU.add)

        for li in range(n_layers):
            l = first_layer + li
            last = (li == n_layers - 1)
            if li > 0:
                dma("sp", GTA, gtpre_d[l, 0], "p_gta")
            dma("sp", GTF, gtpre_d[l, 1], "p_gtf")
            dma("sp", CONVW.rearrange("p a b -> p (a b)"), convw_d[l], "p_convw")
            dma("sp", PSCALE, pscale_d[l], "p_pscale")
            dma("pool", POOLW.rearrange("p a b -> p (a b)"), poolw_d[l], "p_poolw")
            dma("sp", GBC, gpost_d[l, 0:1, :].partition_broadcast(128), "p_gbc")
            for j in range(4):
                dma("pool", WCP[:, 2 * j:2 * j + 2, :].rearrange("p a b -> p (a b)"), wcp_d[l, j], "w_ring")
            if li == 0:
                for i in range(4, NT):
                    dma("pool", X[:, i, :], x_d[i * 128:(i + 1) * 128, :], "x%d" % i)

            for m in range(2):
                memset("pool", UCV[m][:, 0:16], 0.0)
                memset("pool", UPL[m][:, 0:16], 0.0)
            ht_dst = lambda i: H[:, :, i * 128:(i + 1) * 128]
            prenorm(range(0, 4), GTA, ht_dst, XS_A, JUNK_A)
            for q in range(4):
                tok = slice(q * 512, (q + 1) * 512)
                par = q % 2

                def proj(j):
                    b = nb()
                    for kc in range(8):
                        mm(ps[:, b, :], WCP[:, kc, j * 128:(j + 1) * 128], H[:, kc, tok], kc == 0, kc == 7)
                    return ps[:, b, :]

                def conv_seg(m):
                    TA, TB, TC = CSC[par][m]
                    pc = proj(4 + m)
                    copy("act", TA[:, 16:528], pc)
                    ph = proj(0 + m)
                    tt("dve", UCV[m][:, 16:528], ph, TA[:, 16:528], ALU.mult)
                    pbb = proj(2 + m)
                    copy("act", TB[:, 16:528], pbb)
                    ts("dve", TC[:, 16:528], UCV[m][:, 14:526], CONVW[:, m, 0:1], None, ALU.mult)
                    stt(TC[:, 16:528], UCV[m][:, 15:527], CONVW[:, m, 1:2], TC[:, 16:528], ALU.mult, ALU.add)
                    stt(TC[:, 16:528], UCV[m][:, 16:528], CONVW[:, m, 2:3], TC[:, 16:528], ALU.mult, ALU.add)
                    tt("dve", CONCAT[:, 4 + m, tok], TB[:, 16:528], TC[:, 16:528], ALU.mult)
                    if q < 3:
                        copy("pool", UCV[m][:, 0:16], UCV[m][:, 512:528])

                def pool_front(m):
                    TA, TB = PSC[m]
                    pu = proj(6 + m)
                    copy("act", UPL[m][:, 16:528], pu)
                    up = UPL[m]
                    tt("pool", TA[:, 1:528], up[:, 1:528], up[:, 0:527], ALU.add)
                    tt("pool", TB[:, 3:528], TA[:, 3:528], TA[:, 1:526], ALU.add)
                    if m == 1:
                        tt("pool", TA[:, 7:528], TB[:, 7:528], TB[:, 3:524], ALU.add)
                        tt("pool", TB[:, 15:528], TA[:, 15:528], TA[:, 7:520], ALU.add)

                def pool_back(m):
                    TA, TB = PSC[m]
                    up = UPL[m]
                    wlo, whi = (2.0, 4.0) if m == 0 else (8.0, 16.0)
                    pl = PLB[par][m]
                    stt(pl[0:64, :], TA[0:64, 16:528], 1.0 / wlo, up[0:64, 16:528], ALU.mult, ALU.subtract)
                    stt(pl[64:128, :], TB[64:128, 16:528], 1.0 / whi, up[64:128, 16:528], ALU.mult, ALU.subtract)
                    if q == 0:
                        for lo, hi, src in ((0, 64, TA), (64, 128, TB)):
                            tt("dve", TD[lo:hi, 0:16], src[lo:hi, 16:32], RC[lo:hi, m, :], ALU.mult)
                            tt("dve", pl[lo:hi, 0:16], TD[lo:hi, 0:16], up[lo:hi, 16:32], ALU.subtract)
                    b = nb()
                    mm(ps[:, b, :], POOLW[:, m, :], pl, True, True)
                    ts("dve", CONCAT[:, 6 + m, tok], ps[:, b, :], PSCALE[:, m:m + 1], None, ALU.mult)
                    if q < 3:
                        copy("pool", up[:, 0:16], up[:, 512:528])

                segs = [lambda: (pool_front(0), pool_front(1)), lambda: conv_seg(0), lambda: conv_seg(1),
                        lambda: (pool_back(0), pool_back(1))]
                for si, seg in enumerate(segs):
                    nt_ = 4 * (q + 1) + si
                    if q == 0 and si == 0:
                        xs_pend = prenorm_front(nt_, XS_A, JUNK_A)
                    seg()
                    if q < 3:
                        prenorm_back(nt_, xs_pend, GTA, ht_dst)
                        if nt_ + 1 < NT:
                            xs_pend = prenorm_front(nt_ + 1, XS_A, JUNK_A)
            dump("hT", H.rearrange("p a b -> p (a b)"))

            dma("sp", KT[0][64:76, :], kaug_d[:, :], "c_kaug0")
            dma("sp", KT[1][64:76, :], kaug_d[:, :], "c_kaug1")
            dma("sp", KT2[0][64:76, :], kaug_d[:, :], "c_kaug2")
            dma("sp", KT2[1][64:76, :], kaug_d[:, :], "c_kaug3")

            for j in range(2):
                dma("pool", WV[:, 4 * j:4 * j + 4, :].rearrange("p a b -> p (a b)"), wv_d[l, j], "w_ring")
            dma("pool", WQK[0].rearrange("p a b c -> p (a b c)"), wqk_d[l, 0], "w_qk0")
            dma("pool", WQK[1].rearrange("p a b c -> p (a b c)"), wqk_d[l, 1], "w_qk1")
            memset("pool", V[:, :, :, 64:128], 1.0)
            for i in range(NT):
                b = nb()
                for kc in range(8):
                    mm(ps[:, b, :], H[:, kc, i * 128:(i + 1) * 128], WV[:, kc, :], kc == 0, kc == 7)
                src = ps[:, b, :].rearrange("p (a b c) -> p a b c", b=2, c=64)
                copy(alt(), V[:, i, :, 0:64], src[:, :, 0, :])
                copy(alt(), V[:, i, :, 128:192], src[:, :, 1, :])

            SB_, OB_, MB_ = (0, 1, 2, 5), (3, 4), (6, 7)
            sc = [0]
            oc = [0]
            mc = [0]
            ptc = [0]
            rdc = [0]

            def mb():
                b = MB_[mc[0] % 2]
                mc[0] += 1
                return b

            def proj_groups(p):
                wq = WQK[p % 2]
                QTp, KTp = QKSETS[p % 2]
                groups = []

                def setup():
                    if p >= 2:
                        dma("pool", wq.rearrange("p a b c -> p (a b c)"), wqk_d[l, p], "w_qk%d" % (p % 2))
                    for hp in range(2):
                        dma("sp", QTp[hp][64:76, :], qaug_d[2 * p + hp], "c_qaug%d" % (2 * (p % 2) + hp))
                for tc in range(4):
                    tk = slice(tc * 512, (tc + 1) * 512)

                    def gq(tk=tk):
                        b = mb()
                        for kc in range(8):
                            mm(ps[:, b, :], wq[:, 0, kc, :], H[:, kc, tk], kc == 0, kc == 7)
                        ts("dve", QTp[0][0:64, tk], ps[0:64, b, :], 0.125, None, ALU.mult)
                        ts("dve", QTp[1][0:64, tk], ps[64:128, b, :], 0.125, None, ALU.mult)

                    def gk(tk=tk):
                        b = mb()
                        for kc in range(8):
                            mm(ps[:, b, :], wq[:, 1, kc, :], H[:, kc, tk], kc == 0, kc == 7)
                        copy("dve", KTp[0][0:64, tk], ps[0:64, b, :])
                        copy("dve", KTp[1][0:64, tk], ps[64:128, b, :])
                    groups += [gq, gk]
                return setup, groups

            su0, g0 = proj_groups(0)
            su0()
            for g_ in g0:
                g_()
            for p in range(4):
                QT_, KT_ = QKSETS[p % 2]
                nxt = []
                if p < 3:
                    su_, nxt = proj_groups(p + 1)
                    su_()
                nxt = list(nxt)
                for hp in range(2):
                    kmf = KMF[hp]
                    T.op("dve", lambda e, kmf=kmf, kT=KT_[hp]: e.tensor_reduce(
                        out=kmf[0:64, :], in_=kT[0:64, :].rearrange("p (n s) -> p n s", s=256), op=ALU.add, axis=AX.X),
                        reads=[KT_[hp][0:64, :]], writes=[kmf[0:64, :]])
                    copy("dve", KMT[hp][0:64, :], kmf[0:64, :])

                def gate_part1(hp):
                    qT, kmt = QT_[hp], KMT[hp]
                    gb = mb()
                    for j in range(8):
                        mm(ps[:, gb, j * 8:(j + 1) * 8], qT[0:64, (8 + j) * 128:(9 + j) * 128], kmt[0:64, :], True, True)
                    tt("dve", GM, ps[:, gb, 0:64], SBIAS, ALU.add)
                    for j in range(8):
                        T.op("dve", lambda e, j=j: e.max(out=TOP8[:, j * 8:(j + 1) * 8], in_=GM[:, j * 8:(j + 1) * 8]),
                             reads=[GM[:, j * 8:(j + 1) * 8]], writes=[TOP8[:, j * 8:(j + 1) * 8]])
                    thr = TOP8.rearrange("p (a b) -> p a b", b=8)[:, :, 3:4].to_broadcast([128, 8, 8])
                    T.op("dve", lambda e, thr=thr: e.tensor_tensor(
                        out=SELB.rearrange("p (a b) -> p a b", b=8), in0=GM.rearrange("p (a b) -> p a b", b=8),
                        in1=thr, op=ALU.is_ge), reads=[GM, TOP8], writes=[SELB])
                    ts("dve", BIASB[:, 64:128], SELB, -1.0, -NEG, ALU.add, ALU.mult)

                def gate_part2(hp):
                    qT = QT_[hp]
                    for r in range(2):
                        b = mb()
                        for jj in range(4):
                            j = 4 * r + jj
                            mm(ps[0:72, b, jj * 128:(jj + 1) * 128], BIASB[:, j * 8:j * 8 + 72], IDENT, True, True)
                        copy("act" if r == 0 else "dve", qT[64:72, 1024 + 512 * r:1536 + 512 * r], ps[64:72, b, :])

                items = []
                for hp in range(2):
                    for c in range(4):
                        steps = []
                        for kt in range(4 * c + 4):
                            kb = kt // 2
                            if kb < 2 * c:
                                steps.append((kt, 0, 512, None))
                            elif kb == 2 * c:
                                steps.append((kt, 0, 512, 0) if kt == 4 * c else (kt, 128, 512, 128))
                            else:
                                steps.append((kt, 256, 512, 256) if kt == 4 * c + 2 else (kt, 384, 512, 384))
                        ob = OB_[oc[0] % 2]
                        oc[0] += 1
                        for si, st_ in enumerate(steps):
                            items.append((hp, c, ob, st_, si == 0, si == len(steps) - 1))
                LA = 3
                pts = {}
                for s_ in range(len(items) + LA):
                    if s_ < len(items):
                        hp, c, ob, (kt, q0, q1, tri), first, lastst = items[s_]
                        qT, kT = QT_[hp], KT_[hp]
                        Sb = ps[:, SB_[sc[0] % 4], :]
                        sc[0] += 1
                        mm(Sb[:, q0:q1], kT[0:76, kt * 128:(kt + 1) * 128], qT[0:76, c * 512 + q0:c * 512 + q1],
                           True, tri is None)
                        if tri is not None:
                            mm(Sb[:, tri:tri + 128], IDENT, TRI, False, True)
                        pt = PT[ptc[0] % 4]
                        ptc[0] += 1
                        act(pt[:, q0:q1], Sb[:, q0:q1], AF.Exp)
                        pts[s_] = pt
                        if nxt and s_ % 9 == 8:
                            nxt.pop(0)()
                    if s_ - LA >= 0:
                        hp, c, ob, (kt, q0, q1, tri), first, lastst = items[s_ - LA]
                        hd = 2 * p + hp
                        O = ps[:, ob, :]
                        pt = pts.pop(s_ - LA)
                        mm(O[:, q0:q1], vaug(kt, hd), pt[:, q0:q1], first, lastst)
                        if lastst:
                            rd = RDEN[rdc[0] % 2]
                            rdc[0] += 1
                            nlo, dlo = (0, 64) if hp == 0 else (64, 0)
                            if c % 2 == 0:
                                T.op("dve", lambda e, rd=rd, O=O, nlo=nlo, dlo=dlo: e.reciprocal(
                                    out=rd[nlo:nlo + 64, :], in_=O[dlo:dlo + 64, :]),
                                    reads=[O[dlo:dlo + 64, :]], writes=[rd[nlo:nlo + 64, :]])
                            else:
                                act(rd[nlo:nlo + 64, :], O[dlo:dlo + 64, :], AF.Ln)
                                act(rd[nlo:nlo + 64, :], rd[nlo:nlo + 64, :], AF.Exp, scale=-1.0)
                            tt("dve", CONCAT[nlo:nlo + 64, p, c * 512:(c + 1) * 512], O[nlo:nlo + 64, :], rd[nlo:nlo + 64, :], ALU.mult)
                            if c == 0:
                                gate_part1(hp)
                            if c == 1:
                                gate_part2(hp)
                while nxt:
                    nxt.pop(0)()
                if p == 2:
                    for j in range(4):
                        dma("pool", WOUT[:, 2 * j:2 * j + 2, :].rearrange("p a b -> p (a b)"), wout_d[l, j], "w_ring")
            dump("concat", CONCAT.rearrange("p a b -> p (a b)"))

            for j in range(11):
                dma("pool", WD[:, 2 * j:2 * j + 2, :].rearrange("p a b -> p (a b)"), wd_d[l, j], "w_wd")

            hf_dst = lambda i: HF[:, :, (i % 8) * 128:(i % 8 + 1) * 128]
            bank_of = {}
            xs_of = {}
            for i in range(NT + 4):
                if i < NT:
                    banks = (nb(), nb())
                    bank_of[i] = banks
                    for n, b in enumerate(banks):
                        for kc in range(8):
                            mm(ps[:, b, :], CONCAT[:, kc, i * 128:(i + 1) * 128], WOUT[:, kc, n * 512:(n + 1) * 512], kc == 0, kc == 7)
                j3 = i - 3
                if 0 <= j3 < 8:
                    bt = nb()
                    pbt = ps[:, bt, :].bitcast(BF16)
                    for c in range(8):
                        tr(pbt[:, c * 128:(c + 1) * 128], xs_of[j3][:, c * 128:(c + 1) * 128])
                j1 = i - 1
                if 0 <= j1 < NT:
                    for n, b in enumerate(bank_of[j1]):
                        tmp = TMP[n]
                        tt("dve", tmp, ps[:, b, :], GBC[:, n * 512:(n + 1) * 512], ALU.mult)
                        xv = X[:, j1, n * 512:(n + 1) * 512]
                        stt(xv, tmp, RSTD2[:, j1:j1 + 1], xv, ALU.mult, ALU.add)
                j2 = i - 2
                if 0 <= j2 < 8:
                    n_ = pn_ctr[0]
                    pn_ctr[0] += 1
                    xs = XS_F[n_ % 2]
                    act(xs, X[:, j2, :], AF.Copy, scale=RSTD[:, j2:j2 + 1])
                    xs_of[j2] = xs
                if i < NT:
                    for n, b in enumerate(bank_of[i]):
                        act(JUNK_F[n][:, 0:512], ps[:, b, :], AF.Square, scale=1.0 / 32.0, accum=SS2[:, 2 * i + n:2 * i + n + 1])
                    tt("dve", MS2[:, i:i + 1], SS2[:, 2 * i:2 * i + 1], SS2[:, 2 * i + 1:2 * i + 2], ALU.add)
                    act(MS2[:, i:i + 1], MS2[:, i:i + 1], AF.Ln, bias=EPSB[:, 0:1])
                    act(RSTD2[:, i:i + 1], MS2[:, i:i + 1], AF.Exp, scale=-0.5)
                if 0 <= j1 < 8:
                    act(JUNK_F[j1 % 2], X[:, j1, :], AF.Square, scale=1.0 / 32.0, accum=SS[:, j1:j1 + 1])
                    act(MS[:, j1:j1 + 1], SS[:, j1:j1 + 1], AF.Ln, bias=EPSB[:, 0:1])
                    act(RSTD[:, j1:j1 + 1], MS[:, j1:j1 + 1], AF.Exp, scale=-0.5)
                if 0 <= j3 < 8:
                    tt("dve", hf_dst(j3), pbt.rearrange("p (c t) -> p c t", t=128),
                       GTF.unsqueeze(2).to_broadcast([128, 8, 128]), ALU.mult)
            dump("xmid", mem[:, X_OFF:X_OFF + 65536].bitcast(F32))
            dma("sp", GBC, gpost_d[l, 1:2, :].partition_broadcast(128), "p_gbc")

            gu_next = [0]

            def gu_prefetch(upto):
                while gu_next[0] <= upto:
                    g = gu_next[0]
                    dma("pool", GU[g % NS].rearrange("p a b c -> p (a b c)"), wgu_d[l, g % NF], "w_gu%d" % (g % NS))
                    gu_next[0] += 1

            for hh in range(2):
                sgc = 0
                for f in range(NF):
                    g = hh * NF + f
                    gu_prefetch(min(g + NS - 1, 2 * NF - 1))
                    slot = GU[g % NS]
                    for t2 in range(2):
                        tk = slice(t2 * 512, (t2 + 1) * 512)
                        bg, bu = nb(), nb()
                        for kc in range(8):
                            mm(ps[:, bg, :], slot[:, 0, kc, :], HF[:, kc, tk], kc == 0, kc == 7)
                        for kc in range(8):
                            mm(ps[:, bu, :], slot[:, 1, kc, :], HF[:, kc, tk], kc == 0, kc == 7)
                        sg = SG[sgc % 2]
                        sgc += 1
                        act(sg, ps[:, bg, :], AF.Silu)
                        tt("dve", ACTT[:, f, tk], ps[:, bu, :], sg, ALU.mult)
                for i8 in range(8):
                    gi = hh * 8 + i8
                    xsb = prenorm_front(8 + i8, XS_F, JUNK_F) if hh == 0 else None
                    banks = (nb(), nb())
                    for n, b in enumerate(banks):
                        for f in range(NF):
                            mm(ps[:, b, :], ACTT[:, f, i8 * 128:(i8 + 1) * 128], WD[:, f, n * 512:(n + 1) * 512], f == 0, f == NF - 1)
                    if hh == 0:
                        prenorm_back(8 + i8, xsb, GTF, hf_dst)
                    post_norm_residual(gi, banks)
                    if last:
                        out_tok = dma("sp", y_d[gi * 128:(gi + 1) * 128, :], X[:, gi, :], "out")

        T.wait_all("sp", [out_tok])
        T.emit()
        nc._trk_stats = T.stats
    return nc


def _consts():
    bf = ml_dtypes.bfloat16
    ident = np.eye(128, dtype=np.float32).astype(bf)
    s_idx = np.arange(128)[:, None]
    t_idx = np.arange(128)[None, :]
    tri = np.where(s_idx <= t_idx, 0.0, NEG).astype(np.float32).astype(bf)
    pos = np.arange(S)
    blk = pos // 256
    rem = pos % 256
    kaug = np.zeros((12, S), np.float32)
    for j in range(8):
        kaug[j] = (blk == j)
    kaug[8] = 1.0
    kaug[9] = 1.0
    kaug[10] = 256.0 * blk
    kaug[11] = rem
    qaug = np.zeros((8, 12, S), np.float32)
    for h in range(8):
        slope = 2.0 ** (-(h + 1))
        qaug[h, 8] = -slope * 256.0 * blk
        qaug[h, 9] = -slope * rem
        qaug[h, 10] = slope
        qaug[h, 11] = slope
    sbias = np.zeros((8, 8), np.float32)
    for j in range(8):
        cur = (8 + j) // 2
        for n in range(8):
            sbias[j, n] = 0.0 if n < cur else (1e30 if n == cur else -1e30)
    sbias = np.broadcast_to(sbias.reshape(1, 64), (128, 64)).copy()
    rc = np.zeros((128, 2, 16), np.float32)
    wins = (2, 4, 8, 16)
    for pp in range(128):
        for m in range(2):
            w = wins[2 * m + pp // 64]
            rc[pp, m] = 1.0 / np.minimum(np.arange(16) + 1, w)
    return dict(ident=ident, tri=tri, kaug=kaug.astype(bf), qaug=qaug.astype(bf), sbias=sbias,
                rc=rc.reshape(128, 32))


def _prep_weights(w_in, w_out, conv_w, pool_w, pool_scale, g_pre_mix, g_post_mix, g_pre_ffn, g_post_ffn,
                  w_gate, w_up, w_down):
    L = DEPTH
    f32 = np.float32
    w_in = np.asarray(w_in, f32)
    win_r = w_in.reshape(L, 8, 128, 2560).transpose(0, 2, 1, 3)
    wcp = win_r[:, :, :, 1536:2560].reshape(L, 128, 4, 2, 1024).transpose(0, 2, 1, 3, 4).reshape(L, 4, 128, 2048)
    wv = win_r[:, :, :, 1024:1536].reshape(L, 128, 2, 4, 512).transpose(0, 2, 1, 3, 4).reshape(L, 2, 128, 2048)
    wq = win_r[:, :, :, 0:512].reshape(L, 128, 8, 4, 128)
    wk = win_r[:, :, :, 512:1024].reshape(L, 128, 8, 4, 128)
    wqk = np.stack([wq, wk], axis=2)
    wqk = wqk.transpose(0, 4, 1, 2, 3, 5).reshape(L, 4, 128, 2048)
    wout = np.asarray(w_out, f32).reshape(L, 8, 128, 1024).transpose(0, 2, 1, 3)
    wout = wout.reshape(L, 128, 4, 2, 1024).transpose(0, 2, 1, 3, 4).reshape(L, 4, 128, 2048)
    wg = np.asarray(w_gate, f32).reshape(L, 8, 128, NF, 128)
    wu = np.asarray(w_up, f32).reshape(L, 8, 128, NF, 128)
    wgu = np.stack([wg, wu], axis=1)
    wgu = wgu.transpose(0, 4, 3, 1, 2, 5).reshape(L, NF, 128, 2048)
    wd = np.asarray(w_down, f32).reshape(L, 11, 2, 128, 1024).transpose(0, 1, 3, 2, 4).reshape(L, 11, 128, 2048)
    gpost = np.stack([np.asarray(g_post_mix, f32), np.asarray(g_post_ffn, f32)], axis=1)
    gtpre = np.stack([np.asarray(g_pre_mix, f32).reshape(L, 8, 128).transpose(0, 2, 1),
                      np.asarray(g_pre_ffn, f32).reshape(L, 8, 128).transpose(0, 2, 1)], axis=1)
    convw = np.asarray(conv_w, f32).transpose(0, 2, 1).reshape(L, 2, 128, 3).transpose(0, 2, 1, 3).reshape(L, 128, 6)
    pscale = np.asarray(pool_scale, f32).reshape(L, 2, 128).transpose(0, 2, 1)
    pw = np.asarray(pool_w, f32)
    poolw = np.zeros((L, 128, 2, 128), f32)
    for m in range(2):
        poolw[:, 0:64, m, 0:64] = pw[:, 2 * m]
        poolw[:, 64:128, m, 64:128] = pw[:, 2 * m + 1]
    poolw = poolw.reshape(L, 128, 256)
    c = np.ascontiguousarray
    return dict(wcp=c(wcp), wv=c(wv), wqk=c(wqk), wout=c(wout), wgu=c(wgu), wd=c(wd), gpost=c(gpost),
                gtpre=c(gtpre), convw=c(convw), pscale=c(pscale), poolw=c(poolw))


_NC_CACHE = {}


def kernel(x, w_in, w_out, conv_w, pool_w, pool_scale, g_pre_mix, g_post_mix, g_pre_ffn, g_post_ffn,
           w_gate, w_up, w_down):
    x = np.asarray(x, np.float32)
    shared = _prep_weights(w_in, w_out, conv_w, pool_w, pool_scale, g_pre_mix, g_post_mix, g_pre_ffn,
                           g_post_ffn, w_gate, w_up, w_down)
    shared.update(_consts())
    if "nc" not in _NC_CACHE:
        _NC_CACHE["nc"] = build_nc()
    nc = _NC_CACHE["nc"]
    in_maps = []
    for b in range(8):
        m = dict(shared)
        m["x"] = np.ascontiguousarray(x[b])
        in_maps.append(m)
    res = run_bass_kernel_spmd(nc, in_maps, core_ids=list(range(8)))
    return np.stack([np.asarray(r["y"], np.float32) for r in res.results], axis=0)
```
